# Optimizing a Trainium2 kernel written in Bass

```python
import math
import jax, jax.numpy as jnp
from jax import lax
import numpy as np

D_MODEL = 2048
BATCH = 1
SEQ = 8192
DEPTH = 4

N_MIXERS = 2
N_HEADS = 16
HEAD_DIM = 128
BRANCH = N_HEADS * HEAD_DIM
Q_RANK = 512
KV_RANK = 256
IDX_HEADS = 16
IDX_DIM = 128
TOPK_MAX = 256
DSA_QBLOCK = 128
MOBA_BLOCK = 256
MOBA_TOPK = 3
MOBA_QCHUNK = 32
LN_EPS = 1e-5
DN_ALPHA = float((2 * DEPTH) ** 0.25)
DN_BETA = float((8 * DEPTH) ** -0.25)
N_DSA = (DEPTH + 1) // 2
N_MOBA = DEPTH // 2
DSA_IN = Q_RANK + KV_RANK + IDX_DIM + IDX_HEADS + BRANCH
DSA_SPLITS = (Q_RANK, Q_RANK + KV_RANK, Q_RANK + KV_RANK + IDX_DIM, Q_RANK + KV_RANK + IDX_DIM + IDX_HEADS)
MOBA_IN = 4 * BRANCH

kernel_name = "hybrid_dsa_moba_deepnorm_adaln"


def alibi_slopes():
    return jnp.asarray(2.0 ** (-8.0 * np.arange(1, N_HEADS + 1) / N_HEADS), dtype=jnp.float32)


def layer_norm(x, g, b):
    xf = x.astype(jnp.float32)
    mu = jnp.mean(xf, axis=-1, keepdims=True)
    var = jnp.mean(jnp.square(xf - mu), axis=-1, keepdims=True)
    y = (xf - mu) * lax.rsqrt(var + LN_EPS)
    return (y * g.astype(jnp.float32) + b.astype(jnp.float32)).astype(x.dtype)


def rms_norm(x, g):
    xf = x.astype(jnp.float32)
    y = xf * lax.rsqrt(jnp.mean(jnp.square(xf), axis=-1, keepdims=True) + LN_EPS)
    return (y * g.astype(jnp.float32)).astype(x.dtype)


def dsa_mixer(h, w_in, g_q, g_kv, w_uq, w_qi, w_uk, w_uv, w_o):
    B, S, _ = h.shape
    proj = h @ w_in
    c_q, c_kv, k_idx, w_idx, gate = jnp.split(proj, DSA_SPLITS, axis=-1)
    c_q = rms_norm(c_q, g_q)
    c_kv = rms_norm(c_kv, g_kv)
    q = (c_q @ w_uq).reshape(B, S, N_HEADS, HEAD_DIM)
    q_lat = jnp.einsum('bshd,hdc->bshc', q, w_uk)
    q_idx = (c_q @ w_qi).reshape(B, S, IDX_HEADS, IDX_DIM).astype(jnp.float32)
    k_idx = k_idx.astype(jnp.float32)
    w_idx = w_idx.astype(jnp.float32) * (IDX_HEADS ** -0.5)
    topk = min(TOPK_MAX, S // 4)
    slopes = alibi_slopes()
    scale = HEAD_DIM ** -0.5
    s_pos = jnp.arange(S, dtype=jnp.int32)
    b_idx = jnp.arange(B)[:, None, None]

    def block(qb):
        start = qb * DSA_QBLOCK
        qi = lax.dynamic_slice_in_dim(q_idx, start, DSA_QBLOCK, axis=1)
        wi = lax.dynamic_slice_in_dim(w_idx, start, DSA_QBLOCK, axis=1)
        ql = lax.dynamic_slice_in_dim(q_lat, start, DSA_QBLOCK, axis=1)
        t = start + jnp.arange(DSA_QBLOCK, dtype=jnp.int32)
        logits = jnp.einsum('bqhd,bsd->bhqs', qi, k_idx) * (IDX_DIM ** -0.5)
        score = jnp.einsum('bhqs,bqh->bqs', jax.nn.relu(logits), wi)
        causal = s_pos[None, :] <= t[:, None]
        score = jnp.where(causal[None], score, -jnp.inf)
        _, sel = lax.top_k(score, topk)
        kv_sel = c_kv[b_idx, sel]
        att = jnp.einsum('bqhc,bqkc->bhqk', ql, kv_sel).astype(jnp.float32) * scale
        dist = (t[None, :, None] - sel)[:, None]
        att = att - slopes[None, :, None, None] * dist.astype(jnp.float32)
        att = jnp.where(dist >= 0, att, -jnp.inf)
        p = jax.nn.softmax(att, axis=-1).astype(kv_sel.dtype)
        return jnp.einsum('bhqk,bqkc->bqhc', p, kv_sel)

    o_lat = lax.map(block, jnp.arange(S // DSA_QBLOCK))
    o_lat = o_lat.transpose(1, 0, 2, 3, 4).reshape(B, S, N_HEADS, KV_RANK)
    o = jnp.einsum('bshc,hcd->bshd', o_lat, w_uv).reshape(B, S, BRANCH)
    return (o * jax.nn.silu(gate)) @ w_o


def moba_mixer(h, w_in, w_o):
    B, S, _ = h.shape
    proj = h @ w_in
    q, k, v, gate = jnp.split(proj, 4, axis=-1)
    q = q.reshape(B, S, N_HEADS, HEAD_DIM).transpose(0, 2, 1, 3)
    k = k.reshape(B, S, N_HEADS, HEAD_DIM).transpose(0, 2, 1, 3)
    v = v.reshape(B, S, N_HEADS, HEAD_DIM).transpose(0, 2, 1, 3)
    nb = -(-S // MOBA_BLOCK)
    pad = nb * MOBA_BLOCK - S
    kp = jnp.pad(k, ((0, 0), (0, 0), (0, pad), (0, 0)))
    vp = jnp.pad(v, ((0, 0), (0, 0), (0, pad), (0, 0)))
    kb = kp.reshape(B, N_HEADS, nb, MOBA_BLOCK, HEAD_DIM)
    vb = vp.reshape(B, N_HEADS, nb, MOBA_BLOCK, HEAD_DIM)
    k_mean = jnp.mean(kb.astype(jnp.float32), axis=3)
    k_sel = max(1, min(MOBA_TOPK, nb - 1))
    slopes = alibi_slopes()[None, :, None, None]
    scale = HEAD_DIM ** -0.5
    blk_ids = jnp.arange(nb, dtype=jnp.int32)
    bi = jnp.arange(B)[:, None, None, None]
    hi = jnp.arange(N_HEADS)[None, :, None, None]
    in_blk = jnp.arange(MOBA_BLOCK, dtype=jnp.int32)

    def chunk(ci):
        start = ci * MOBA_QCHUNK
        qc = lax.dynamic_slice_in_dim(q, start, MOBA_QCHUNK, axis=2)
        t = start + jnp.arange(MOBA_QCHUNK, dtype=jnp.int32)
        own = start // MOBA_BLOCK
        g = jnp.einsum('bhqd,bhnd->bhqn', qc.astype(jnp.float32), k_mean)
        g = jnp.where(blk_ids < own, g, -jnp.inf)
        _, sel = lax.top_k(g, k_sel)
        ksel = kb[bi, hi, sel]
        vsel = vb[bi, hi, sel]
        s_past = jnp.einsum('bhqd,bhqnkd->bhqnk', qc, ksel).astype(jnp.float32) * scale
        pos_past = sel[..., None] * MOBA_BLOCK + in_blk
        d_past = (t[None, None, :, None, None] - pos_past).astype(jnp.float32)
        s_past = s_past - slopes[..., None] * d_past
        s_past = jnp.where((sel < own)[..., None], s_past, -jnp.inf)
        s_past = s_past.reshape(B, N_HEADS, MOBA_QCHUNK, k_sel * MOBA_BLOCK)
        k_own = lax.dynamic_slice_in_dim(kp, own * MOBA_BLOCK, MOBA_BLOCK, axis=2)
        v_own = lax.dynamic_slice_in_dim(vp, own * MOBA_BLOCK, MOBA_BLOCK, axis=2)
        s_own = jnp.einsum('bhqd,bhkd->bhqk', qc, k_own).astype(jnp.float32) * scale
        d_own = t[:, None] - (own * MOBA_BLOCK + in_blk)[None, :]
        s_own = s_own - slopes * d_own.astype(jnp.float32)[None, None]
        s_own = jnp.where((d_own >= 0)[None, None], s_own, -jnp.inf)
        p = jax.nn.softmax(jnp.concatenate([s_past, s_own], axis=-1), axis=-1).astype(v.dtype)
        p_past = p[..., :k_sel * MOBA_BLOCK]
        p_own = p[..., k_sel * MOBA_BLOCK:]
        vsel = vsel.reshape(B, N_HEADS, MOBA_QCHUNK, k_sel * MOBA_BLOCK, HEAD_DIM)
        return (jnp.einsum('bhqm,bhqmd->bhqd', p_past, vsel)
                + jnp.einsum('bhqk,bhkd->bhqd', p_own, v_own))

    out = lax.map(chunk, jnp.arange(S // MOBA_QCHUNK))
    out = out.transpose(1, 0, 3, 2, 4).reshape(B, S, BRANCH)
    return (out * jax.nn.silu(gate)) @ w_o


def setup_inputs(seed: int = 0) -> dict:
    key = jax.random.key(seed)
    ks = jax.random.split(key, 20)
    f32 = jnp.float32
    nrm = lambda k, shape, s: jax.random.normal(k, shape, f32) * s
    return {
        "x": nrm(ks[0], (BATCH, SEQ, D_MODEL), 1.0),
        "c": nrm(ks[1], (BATCH, D_MODEL), 1.0),
        "ada_w": nrm(ks[2], (DEPTH, D_MODEL, 3 * D_MODEL), D_MODEL ** -0.5),
        "ada_b": nrm(ks[3], (DEPTH, 3 * D_MODEL), 0.02),
        "ln_g": 1.0 + nrm(ks[4], (DEPTH, D_MODEL), 0.02),
        "ln_b": nrm(ks[5], (DEPTH, D_MODEL), 0.02),
        "dsa_w_in": nrm(ks[6], (N_DSA, D_MODEL, DSA_IN), D_MODEL ** -0.5),
        "dsa_g_q": 1.0 + nrm(ks[7], (N_DSA, Q_RANK), 0.02),
        "dsa_g_kv": 1.0 + nrm(ks[8], (N_DSA, KV_RANK), 0.02),
        "dsa_w_uq": nrm(ks[9], (N_DSA, Q_RANK, BRANCH), Q_RANK ** -0.5),
        "dsa_w_qi": nrm(ks[10], (N_DSA, Q_RANK, IDX_HEADS * IDX_DIM), Q_RANK ** -0.5),
        "dsa_w_uk": nrm(ks[11], (N_DSA, N_HEADS, HEAD_DIM, KV_RANK), HEAD_DIM ** -0.5),
        "dsa_w_uv": nrm(ks[12], (N_DSA, N_HEADS, KV_RANK, HEAD_DIM), KV_RANK ** -0.5),
        "dsa_w_o": nrm(ks[13], (N_DSA, BRANCH, D_MODEL), DN_BETA * BRANCH ** -0.5),
        "moba_w_in": nrm(ks[14], (N_MOBA, D_MODEL, MOBA_IN), D_MODEL ** -0.5),
        "moba_w_o": nrm(ks[15], (N_MOBA, BRANCH, D_MODEL), DN_BETA * BRANCH ** -0.5),
    }


def reference(x, c, ada_w, ada_b, ln_g, ln_b, dsa_w_in, dsa_g_q, dsa_g_kv, dsa_w_uq, dsa_w_qi,
              dsa_w_uk, dsa_w_uv, dsa_w_o, moba_w_in, moba_w_o):
    c_act = jax.nn.silu(c)
    for i in range(DEPTH):
        mod = c_act @ ada_w[i] + ada_b[i]
        shift, scl, gate = jnp.split(mod, 3, axis=-1)
        h = x * (1.0 + scl[:, None, :]) + shift[:, None, :]
        j = i // N_MIXERS
        if i % N_MIXERS == 0:
            y = dsa_mixer(h, dsa_w_in[j], dsa_g_q[j], dsa_g_kv[j], dsa_w_uq[j], dsa_w_qi[j],
                          dsa_w_uk[j], dsa_w_uv[j], dsa_w_o[j])
        else:
            y = moba_mixer(h, moba_w_in[j], moba_w_o[j])
        x = layer_norm(DN_ALPHA * x + gate[:, None, :] * y, ln_g[i], ln_b[i])
    return x
```

```python
import numpy as np
import ml_dtypes
import concourse.bass as bass
import concourse.mybir as mybir
from concourse.bass_utils import run_bass_kernel_spmd

F32 = mybir.dt.float32
BF16 = mybir.dt.bfloat16
U8 = mybir.dt.uint8
ALU = mybir.AluOpType
AF = mybir.ActivationFunctionType
AX = mybir.AxisListType
NPBF = ml_dtypes.bfloat16

NCORES = 8
S = 8192
D = 2048
T = 1024
NSLOT = 8
H = 16
DEPTH = 4
ALPHA = float((2 * DEPTH) ** 0.25)
LN_EPS = 1e-5
SCALE = 128 ** -0.5
SLOPES = [float(2.0 ** (-8.0 * (h + 1) / 16)) for h in range(16)]
BIGD = 1.0e6
NBIS = 24


class Buf:
    def __init__(self, name, t, kind):
        self.name = name
        self.t = t
        self.kind = kind
        self.wev = {}
        self.rev = {}
        self.dsems = {}

    def __getitem__(self, idx):
        return self.t[idx]


class MK:
    ENG = ("pe", "act", "dve", "pool", "sp")

    def __init__(self, nc, self_sync=True):
        self.nc = nc
        self.self_sync = self_sync
        self.streams = {e: [] for e in self.ENG}
        self.sems = {}
        self.count = {e: 0 for e in self.ENG}
        self.semcount = {}
        self.seen = {e: {} for e in self.ENG}
        self.ctx = []
        self.pctx = []
        self.in_phase = False
        self.free_dsem = {"sp": [], "pool": [], "act": []}
        self.holders = []
        self.ndsem = 0
        self.ncc = 0
        self.outs = []
        for e in ("pe", "act", "dve", "pool"):
            self._mksem("E_" + e)
        self._mksem("CC")
        self.ccdummy = self.sb("ccdummy", [128, 8], F32)

    def _mksem(self, key):
        cm = self.nc.semaphore(key)
        h = cm.__enter__()
        self.ctx.append(cm)
        self.sems[key] = h
        self.semcount[key] = 0
        return key

    def _enter(self, cm):
        t = cm.__enter__()
        (self.pctx if self.in_phase else self.ctx).append(cm)
        return t

    def _uname(self, name):
        self.uid = getattr(self, "uid", 0) + 1
        return "%s_u%d" % (name, self.uid)

    def sb(self, name, shape, dtype):
        return Buf(name, self._enter(self.nc.sbuf_tensor(self._uname(name), list(shape), dtype)), "sb")

    def ps(self, name, shape, dtype=F32):
        return Buf(name, self._enter(self.nc.psum_tensor(self._uname(name), list(shape), dtype)), "ps")

    def dram(self, name, shape, dtype, kind=None):
        if kind is None:
            t = self.nc.dram_tensor(name, list(shape), dtype)
        else:
            t = self.nc.dram_tensor(name, list(shape), dtype, kind=kind)
        b = Buf(name, t.ap(), "dram")
        if kind == "ExternalOutput":
            self.outs.append(b)
        return b

    def din(self, name, shape, dtype):
        return self.dram(name, shape, dtype, kind="ExternalInput")

    def dout(self, name, shape, dtype):
        return self.dram(name, shape, dtype, kind="ExternalOutput")

    def view(self, buf, name):
        return Buf(name, buf.t, buf.kind)

    def _waits(self, eng, reads, writes, skip_key=None):
        need = {}
        for b in reads:
            for k, v in b.wev.items():
                need[k] = max(need.get(k, 0), v)
        for b in writes:
            for k, v in b.wev.items():
                if k != skip_key:
                    need[k] = max(need.get(k, 0), v)
            for k, v in b.rev.items():
                need[k] = max(need.get(k, 0), v)
        out = []
        seen = self.seen[eng]
        for k, v in need.items():
            if k == "E_" + eng and (eng == "pe" or not self.self_sync):
                continue
            if seen.get(k, 0) >= v:
                continue
            seen[k] = v
            out.append((k, v))
        return out

    def _commit(self, ev, reads, writes):
        k, v = ev
        for b in reads:
            b.rev[k] = max(b.rev.get(k, 0), v)
        for b in writes:
            b.wev = {k: v}
            b.rev = {}

    def op(self, eng, fn, reads=(), writes=()):
        waits = self._waits(eng, reads, writes)
        self.count[eng] += 1
        k = "E_" + eng
        self.semcount[k] = self.count[eng]
        self.streams[eng].append((waits, fn, k, 1))
        self._commit((k, self.count[eng]), reads, writes)

    def dma(self, eng, out_ap, in_ap, reads=(), writes=(), **kw):
        tgt = writes[0]
        key = tgt.dsems.get(eng)
        if key is None:
            if self.free_dsem[eng]:
                key = self.free_dsem[eng].pop()
            else:
                self.ndsem += 1
                key = self._mksem("D%d_%s" % (self.ndsem, eng))
            tgt.dsems[eng] = key
            self.holders.append(tgt)
        waits = self._waits(eng, reads, writes, skip_key=key)
        self.semcount[key] += 16
        ev = (key, self.semcount[key])
        fn = lambda h, o=out_ap, i=in_ap, kw=kw: h.dma_start(out=o, in_=i, **kw)
        self.streams[eng].append((waits, fn, key, 16))
        self._commit(ev, reads, writes)

    def allgather(self, src, dst):
        waits = self._waits("pool", [src], [dst])
        self.ncc += 1
        self.semcount["CC"] = self.ncc

        def cc(h):
            return h.collective_compute("AllGather", ALU.bypass, replica_groups=[list(range(NCORES))],
                                        ins=[src.t.opt()], outs=[dst.t.opt()])
        self.streams["pool"].append((waits, cc, "CC", 1))
        self.seen["pool"]["CC"] = self.ncc
        self.count["pool"] += 1
        self.semcount["E_pool"] = self.count["pool"]
        dummy = self.ccdummy
        self.streams["pool"].append(([("CC", self.ncc)], lambda h: h.memset(dummy[:], 0.0), "E_pool", 1))
        self._commit(("E_pool", self.count["pool"]), [src], [dst])

    def barrier(self):
        for e in self.ENG:
            waits = []
            seen = self.seen[e]
            for k, v in self.semcount.items():
                if k != "CC" and v > 0 and seen.get(k, 0) < v:
                    seen[k] = v
                    waits.append((k, v))
            self.streams[e].append((waits, None, None, 0))

    def phase_begin(self):
        self.in_phase = True

    def phase_end(self):
        self.barrier()
        for cm in reversed(self.pctx):
            cm.__exit__(None, None, None)
        self.pctx = []
        for b in self.holders:
            for eng, key in b.dsems.items():
                self.free_dsem[eng].append(key)
            b.dsems = {}
        self.holders = []
        self.in_phase = False

    def finish(self):
        waits = self._waits("sp", self.outs, ())
        self.streams["sp"].append((waits, None, None, 0))
        nc = self.nc
        sems = self.sems
        streams = self.streams

        def replay(h, items, inline=False):
            for waits, fn, sk, amt in items:
                if fn is None:
                    for k, v in waits:
                        h.wait_ge(sems[k], v)
                    continue
                if inline and waits:
                    for k, v in waits[:-1]:
                        h.wait_ge(sems[k], v)
                    k, v = waits[-1]
                    fn(h)._wait_ge(sems[k], v).then_inc(sems[sk], amt)
                else:
                    for k, v in waits:
                        h.wait_ge(sems[k], v)
                    fn(h).then_inc(sems[sk], amt)

        with nc.Block() as block:
            @block.tensor
            def _(h):
                replay(h, streams["pe"], inline=True)

            @block.scalar
            def _(h):
                replay(h, streams["act"])

            @block.vector
            def _(h):
                replay(h, streams["dve"])

            @block.gpsimd
            def _(h):
                replay(h, streams["pool"])

            @block.sync
            def _(h):
                replay(h, streams["sp"])
        for cm in reversed(self.pctx + self.ctx):
            cm.__exit__(None, None, None)
        self.ctx = []
        self.pctx = []


def mm(m, out_b, out_ap, lhsT_b, lhsT_ap, rhs_b, rhs_ap, start, stop):
    m.op("pe", lambda h: h.matmul(out_ap, lhsT=lhsT_ap, rhs=rhs_ap, start=start, stop=stop),
         reads=[lhsT_b, rhs_b], writes=[out_b])


class LinT:
    def __init__(self, m, KCmax, name="lw"):
        self.m = m
        self.slots = [m.sb("%s%d" % (name, i), [128, KCmax, 512], BF16) for i in range(2)]
        self.banks = [m.ps("%sps%d" % (name, i), [128, 512]) for i in range(2)]
        self.nslot = 0
        self.nbank = 0

    def load(self, W, w_ap, f0, nf, KC):
        m = self.m
        slot = self.slots[self.nslot % 2]
        self.nslot += 1
        src = w_ap[:, f0:f0 + nf].rearrange("(kc p) f -> p kc f", p=128)
        half = max(1, KC // 2)
        for a in range(0, KC, half):
            b = min(KC, a + half)
            m.dma("pool", slot[:, a:b, 0:nf], src[:, a:b, :], reads=[W], writes=[slot])
        return slot

    def run(self, W, w_ap, f0, nf, KC, inT, Tn, evac):
        m = self.m
        for fb in range(f0, f0 + nf, 512):
            nb = min(512, f0 + nf - fb)
            slot = self.load(W, w_ap, fb, nb, KC)
            for fs in range(0, nb, 128):
                mr = min(128, nb - fs)
                for t0 in range(0, Tn, 512):
                    tn = min(512, Tn - t0)
                    ps = self.banks[self.nbank % 2]
                    self.nbank += 1
                    for kc in range(KC):
                        mm(m, ps, ps[0:mr, 0:tn], slot, slot[:, kc, fs:fs + mr],
                           inT, inT[:, kc, t0:t0 + tn], kc == 0, kc == KC - 1)
                    evac(fb + fs, mr, t0, tn, ps, ps[0:mr, 0:tn])


def make_ident(m, dtype, name="ident"):
    idf = m.sb(name + "f", [128, 128], F32)
    m.op("pool", lambda h: h.memset(idf[:], 1.0), writes=[idf])
    m.op("pool", lambda h: h.affine_select(out=idf[:], in_=idf[:], pattern=[[-1, 128]],
                                           compare_op=ALU.is_equal, fill=0.0, base=0,
                                           channel_multiplier=1), reads=[idf], writes=[idf])
    if dtype == F32:
        return idf
    idb = m.sb(name + "b", [128, 128], dtype)
    m.op("dve", lambda h: h.tensor_copy(out=idb[:], in_=idf[:]), reads=[idf], writes=[idb])
    return idb


def modulate(m, xT, modsb, l, hT, Tn):
    sh = m.sb("sh", [128, 16], F32)
    sc1 = m.sb("sc1", [128, 16], F32)
    m.op("dve", lambda h: h.tensor_copy(out=sh[:], in_=modsb[:, l, 0:16]), reads=[modsb], writes=[sh])
    m.op("dve", lambda h: h.tensor_scalar(out=sc1[:], in0=modsb[:, l, 16:32], scalar1=1.0, scalar2=None, op0=ALU.add),
         reads=[modsb], writes=[sc1])
    xs = [m.sb("xs%d" % i, [128, 16, 256], F32) for i in range(2)]
    for ci, t0 in enumerate(range(0, Tn, 256)):
        xb = xs[ci % 2]
        m.dma("sp", xb[:, 0:8, :], xT[:, 0:8, t0:t0 + 256], reads=[xT], writes=[xb])
        m.dma("sp", xb[:, 8:16, :], xT[:, 8:16, t0:t0 + 256], reads=[xT], writes=[xb])
        for kc in range(16):
            m.op("dve", lambda h, kc=kc, xb=xb, t0=t0: h.tensor_scalar(
                out=hT[:, kc, t0:t0 + 256], in0=xb[:, kc, :], scalar1=sc1[:, kc:kc + 1],
                scalar2=sh[:, kc:kc + 1], op0=ALU.mult, op1=ALU.add),
                reads=[xb, sc1, sh], writes=[hT])


def rmsnorm_T(m, srcT, KC, nfeat, gT, dstT, Tn, onesf, tagname):
    ps = m.ps(tagname + "ps", [128, 512])
    sq = [m.sb(tagname + "sq%d" % i, [128, 512], F32) for i in range(2)]
    rs = m.sb(tagname + "rs", [128, 512], F32)
    epsb = m.sb(tagname + "eps", [128, 1], F32)
    m.op("dve", lambda h: h.memset(epsb[:], LN_EPS), writes=[epsb])
    for t0 in range(0, Tn, 512):
        for kc in range(KC):
            s = sq[kc % 2]
            m.op("act", lambda h, s=s, kc=kc, t0=t0: h.activation(out=s[:], in_=srcT[:, kc, t0:t0 + 512], func=AF.Square),
                 reads=[srcT], writes=[s])
            mm(m, ps, ps[:, :], onesf, onesf[:, :], s, s[:, :], kc == 0, kc == KC - 1)
        m.op("act", lambda h: h.activation(out=rs[:], in_=ps[:, :], func=AF.Sqrt, bias=epsb[:], scale=1.0 / nfeat),
             reads=[ps, epsb], writes=[rs])
        m.op("dve", lambda h: h.reciprocal(out=rs[:], in_=rs[:]), reads=[rs], writes=[rs])
        for kc in range(KC):
            m.op("dve", lambda h, kc=kc, t0=t0: h.scalar_tensor_tensor(
                out=dstT[:, kc, t0:t0 + 512], in0=srcT[:, kc, t0:t0 + 512], scalar=gT[:, kc:kc + 1],
                in1=rs[:], op0=ALU.mult, op1=ALU.mult), reads=[srcT, gT, rs], writes=[dstT])


def evac_copy(m, eng, dst_b, dst_ap, ps_b, ps_ap, scale=None, func=None):
    if eng == "act":
        f = func if func is not None else AF.Copy
        if scale is None:
            m.op("act", lambda h: h.activation(out=dst_ap, in_=ps_ap, func=f), reads=[ps_b], writes=[dst_b])
        else:
            m.op("act", lambda h: h.activation(out=dst_ap, in_=ps_ap, func=f, scale=scale), reads=[ps_b], writes=[dst_b])
    else:
        if scale is None:
            m.op("dve", lambda h: h.tensor_copy(out=dst_ap, in_=ps_ap), reads=[ps_b], writes=[dst_b])
        else:
            m.op("dve", lambda h: h.tensor_scalar(out=dst_ap, in0=ps_ap, scalar1=scale, scalar2=None, op0=ALU.mult),
                 reads=[ps_b], writes=[dst_b])


class Stager:
    def __init__(self, m, n=4):
        self.m = m
        self.stg = [m.sb("stg%d" % i, [128, 512], BF16) for i in range(n)]
        self.k = 0

    def put(self, dst, dst_ap, ps, ps_ap, mr, eng, func=None, scale=None):
        m = self.m
        s = self.stg[self.k % len(self.stg)]
        self.k += 1
        evac_copy(m, eng, s, s[0:mr, :], ps, ps_ap, scale=scale, func=func)
        m.dma("sp", dst_ap, s[0:mr, :], reads=[s], writes=[dst])


def emit_M(m, E, modsb, SC):
    m.phase_begin()
    cT, aw, ab = E["cT"], E["aw"], E["ab"]
    c_sb = m.sb("c_sb", [128, 16], F32)
    sg = m.sb("sg", [128, 16], F32)
    cact = m.sb("cact", [128, 16, 2], F32)
    ab_sb = m.sb("ab_sb", [128, 24], F32)
    res = m.sb("res", [128, 24], F32)
    ws = [m.sb("w%d" % i, [128, 16, 128], F32) for i in range(2)]
    ps = m.ps("ps", [128, 512])
    m.dma("sp", c_sb[:], cT[:, :], reads=[cT], writes=[c_sb])
    m.dma("sp", ab_sb[:], ab[:, :], reads=[ab], writes=[ab_sb])
    m.op("act", lambda h: h.activation(out=sg[:], in_=c_sb[:], func=AF.Sigmoid), reads=[c_sb], writes=[sg])
    for j in range(2):
        m.op("dve", lambda h, j=j: h.tensor_tensor(out=cact[:, :, j], in0=sg[:], in1=c_sb[:], op=ALU.mult),
             reads=[sg, c_sb], writes=[cact])
    for g in range(24):
        w = ws[g % 2]
        m.dma("sp", w[:, 0:8, :], aw[g, :, 0:8, :], reads=[aw], writes=[w])
        m.dma("sp", w[:, 8:16, :], aw[g, :, 8:16, :], reads=[aw], writes=[w])
        for kc in range(16):
            mm(m, ps, ps[:, 2 * g:2 * g + 2], w, w[:, kc, :], cact, cact[:, kc, :], kc == 0, kc == 15)
    psv = ps[:, 0:48].rearrange("p (g two) -> p g two", two=2)
    m.op("dve", lambda h: h.tensor_tensor(out=res[:], in0=psv[:, :, 0], in1=ab_sb[:], op=ALU.add),
         reads=[ps, ab_sb], writes=[res])
    m.dma("sp", SC["mod_s"][:, :], res[:], reads=[res], writes=[SC["mod_s"]])
    m.allgather(SC["mod_s"], SC["mod_g"])
    gsrc = SC["mod_g"].t.rearrange("(r p) (l q) -> r p l q", p=128, q=6)
    for r in range(NCORES):
        m.dma("sp", modsb[:, :, r * 6:(r + 1) * 6], gsrc[r], reads=[SC["mod_g"]], writes=[modsb])
    m.phase_end()


def emit_P_dsa(m, E, j, l, xT, modsb, SC):
    m.phase_begin()
    w_in, w_uq, w_qi, w_uk = (E[k % j] for k in ("dsa_w_in%d", "dsa_w_uq%d", "dsa_w_qi%d", "dsa_w_uk%d"))
    gqT, gkvT = E["dsa_gqT%d" % j], E["dsa_gkvT%d" % j]
    o_qlat, o_qidx, o_widx, o_sg = SC["qlatT"], SC["qidxT"], SC["widx"], SC["sgT"]
    s_ckv, s_kidx, s_v = SC["s_ckv"], SC["s_kidx"], SC["s_v"]
    hT = m.sb("hT", [128, 16, T], BF16)
    modulate(m, xT, modsb, l, hT, T)
    lin = LinT(m, 16)
    st = Stager(m)
    onesf = m.sb("onesf", [128, 128], F32)
    m.op("pool", lambda h: h.memset(onesf[:], 1.0), writes=[onesf])
    cqT = m.sb("cqT", [128, 4, T], F32)
    ckvT = m.sb("ckvT_s", [128, 2, T], F32)

    def ev_cq(fa, mr, t0, tn, ps, ps_ap):
        evac_copy(m, "act", cqT, cqT[:, fa // 128, t0:t0 + tn], ps, ps_ap)

    def ev_ckv(fa, mr, t0, tn, ps, ps_ap):
        evac_copy(m, "dve", ckvT, ckvT[:, (fa - 512) // 128, t0:t0 + tn], ps, ps_ap)

    def ev_kidx(fa, mr, t0, tn, ps, ps_ap):
        st.put(s_kidx, s_kidx[:, t0:t0 + tn], ps, ps_ap, mr, "dve")

    def ev_gate(fa, mr, t0, tn, ps, ps_ap):
        st.put(o_sg, o_sg[:, (fa - 912) // 128, t0:t0 + tn], ps, ps_ap, mr, "act", func=AF.Silu)

    lin.run(w_in, w_in, 0, 512, 16, hT, T, ev_cq)
    lin.run(w_in, w_in, 512, 256, 16, hT, T, ev_ckv)
    lin.run(w_in, w_in, 768, 128, 16, hT, T, ev_kidx)
    gq = m.sb("gq", [128, 4], F32)
    gkv = m.sb("gkv", [128, 2], F32)
    m.dma("sp", gq[:], gqT[:, :], reads=[gqT], writes=[gq])
    m.dma("sp", gkv[:], gkvT[:, :], reads=[gkvT], writes=[gkv])
    cqn = m.sb("cqn", [128, 4, T], BF16)
    ckvn = m.sb("ckvn", [128, 2, T], BF16)
    rmsnorm_T(m, ckvT, 2, 256.0, gkv, ckvn, T, onesf, "rk")
    for cc in range(2):
        m.dma("sp", s_ckv[cc * 128:(cc + 1) * 128, :], ckvn[:, cc, :], reads=[ckvn], writes=[s_ckv])
    identb = make_ident(m, BF16)
    vt = m.sb("vt", [128, 8, 256], BF16)
    ptp = [m.ps("ptp%d" % i, [128, 512], BF16) for i in range(2)]
    for jj in range(8):
        pp = ptp[jj % 2]
        for cc in range(2):
            m.op("pe", lambda h, jj=jj, cc=cc, pp=pp: h.transpose(pp[:, cc * 128:(cc + 1) * 128], ckvn[:, cc, jj * 128:(jj + 1) * 128], identb[:]),
                 reads=[ckvn, identb], writes=[pp])
        evac_copy(m, "act" if jj % 2 else "dve", vt, vt[:, jj, :], pp, pp[:, 0:256])
    m.dma("sp", s_v.t.rearrange("(j i) c -> i j c", i=128), vt[:], reads=[vt], writes=[s_v])
    m.allgather(s_ckv, SC["g_ckv"])
    m.allgather(s_kidx, SC["g_kidx"])
    m.allgather(s_v, SC["g_v"])
    rmsnorm_T(m, cqT, 4, 512.0, gq, cqn, T, onesf, "rq")
    lin.run(w_in, w_in, 912, 2048, 16, hT, T, ev_gate)
    slot = lin.load(w_in, w_in, 896, 16, 16)
    wst = m.sb("wst", [128, 8, 16], F32)
    for jj in range(8):
        ps = lin.banks[jj % 2]
        for kc in range(16):
            mm(m, ps, ps[:, 0:16], hT, hT[:, kc, jj * 128:(jj + 1) * 128], slot, slot[:, kc, 0:16], kc == 0, kc == 15)
        evac_copy(m, "dve", wst, wst[:, jj, :], ps, ps[:, 0:16])
    m.dma("sp", o_widx[:, :, :], wst[:], reads=[wst], writes=[o_widx])
    qT = m.sb("qT", [128, 16, T], BF16)

    def ev_q(fa, mr, t0, tn, ps, ps_ap):
        evac_copy(m, "act", qT, qT[:, fa // 128, t0:t0 + tn], ps, ps_ap)

    lin.run(w_uq, w_uq, 0, 2048, 4, cqn, T, ev_q)
    wuk = m.sb("wuk", [128, 16, 256], BF16)
    m.dma("pool", wuk[:, 0:8, :], w_uk[0:8].rearrange("h d c -> d h c"), reads=[w_uk], writes=[wuk])
    m.dma("pool", wuk[:, 8:16, :], w_uk[8:16].rearrange("h d c -> d h c"), reads=[w_uk], writes=[wuk])
    nb = 0
    for hh in range(16):
        for cc in range(2):
            for t0 in range(0, T, 512):
                ps = lin.banks[nb % 2]
                nb += 1
                mm(m, ps, ps[:, :], wuk, wuk[:, hh, cc * 128:(cc + 1) * 128], qT, qT[:, hh, t0:t0 + 512], True, True)
                st.put(o_qlat, o_qlat[:, cc, hh, t0:t0 + 512], ps, ps[:, :], 128, "act" if nb % 2 else "dve", scale=SCALE)

    def ev_qi(fa, mr, t0, tn, ps, ps_ap):
        st.put(o_qidx, o_qidx[:, fa // 128, t0:t0 + tn], ps, ps_ap, mr, "dve")

    lin.run(w_qi, w_qi, 0, 2048, 4, cqn, T, ev_qi)
    m.phase_end()


class AttnBufs:
    def __init__(self, m, vw):
        self.m = m
        self.vw = vw
        self.S = [m.ps("S%d" % i, [128, 512]) for i in range(2)]
        self.PT = [m.ps("PT%d" % i, [128, 512], BF16) for i in range(2)]
        self.acc = [m.ps("acc%d" % i, [128, 512]) for i in range(2)]
        self.sc = [m.sb("sc%d" % i, [128, 512], F32) for i in range(2)]
        self.p = [m.sb("p%d" % i, [128, 512], BF16) for i in range(2)]
        self.pt = [m.sb("pt%d" % i, [128, 512], BF16) for i in range(2)]
        self.ost = [m.sb("ost%d" % i, [128, vw], BF16) for i in range(2)]
        self.otr = [m.sb("otr%d" % i, [128, vw], BF16) for i in range(2)]
        self.rden = [m.sb("rden%d" % i, [128, 1], F32) for i in range(2)]
        self.ident = make_ident(m, BF16)
        self.n = 0
        self.na = 0


def attn_tile(m, A, acc, score_fn, dm_b, dm_ap, slope, exp_fn, v_b, v_aps, first, last):
    n = A.n
    A.n += 1
    Sb, sc, p, PT, pt = A.S[n % 2], A.sc[n % 2], A.p[n % 2], A.PT[n % 2], A.pt[n % 2]
    score_fn(Sb)
    m.op("dve", lambda h: h.scalar_tensor_tensor(out=sc[:], in0=dm_ap, scalar=slope, in1=Sb[:, :],
                                                 op0=ALU.mult, op1=ALU.add), reads=[dm_b, Sb], writes=[sc])
    exp_fn(sc, p)
    for q in range(4):
        m.op("pe", lambda h, q=q: h.transpose(PT[:, q * 128:(q + 1) * 128], p[:, q * 128:(q + 1) * 128], A.ident[:]),
             reads=[p, A.ident], writes=[PT])
    if n % 2 == 0:
        m.op("act", lambda h: h.activation(out=pt[:], in_=PT[:, :], func=AF.Copy), reads=[PT], writes=[pt])
    else:
        m.op("dve", lambda h: h.tensor_copy(out=pt[:], in_=PT[:, :]), reads=[PT], writes=[pt])
    vw = A.vw
    for q in range(4):
        mm(m, acc, acc[:, 0:vw + 1], pt, pt[:, q * 128:(q + 1) * 128], v_b, v_aps[q], first and q == 0, last and q == 3)


def attn_finish(m, A, acc, oT, hh, j):
    k = A.na
    A.na += 1
    vw = A.vw
    nvc = vw // 128
    rd, ost, otr = A.rden[k % 2], A.ost[k % 2], A.otr[k % 2]
    PT = A.PT[A.n % 2]
    m.op("dve", lambda h: h.reciprocal(out=rd[:], in_=acc[:, vw:vw + 1]), reads=[acc], writes=[rd])
    m.op("act", lambda h: h.activation(out=ost[:], in_=acc[:, 0:vw], func=AF.Copy, scale=rd[:]),
         reads=[acc, rd], writes=[ost])
    for vc in range(nvc):
        m.op("pe", lambda h, vc=vc: h.transpose(PT[:, vc * 128:(vc + 1) * 128], ost[:, vc * 128:(vc + 1) * 128], A.ident[:]),
             reads=[ost, A.ident], writes=[PT])
    m.op("dve", lambda h: h.tensor_copy(out=otr[:], in_=PT[:, 0:vw]), reads=[PT], writes=[otr])
    m.dma("sp", oT[:, hh * nvc:(hh + 1) * nvc, j * 128:(j + 1) * 128], otr[:, :].rearrange("p (v t) -> p v t", t=128),
          reads=[otr], writes=[oT])


def emit_B_dsa(m, E, SC):
    m.phase_begin()
    i_qlat, i_qidx, i_widx = SC["qlatT"], SC["qidxT"], SC["widx"]
    g_ckv, g_kidx, g_v = SC["g_ckv"], SC["g_kidx"], SC["g_v"]
    oT = SC["oT_dsa"]
    ckv = m.sb("ckv", [128, 2, S], BF16)
    vaug = m.sb("vaug", [128, 64, 257], BF16)
    kidx = m.sb("kidx", [128, S], BF16)
    score = m.sb("score", [128, S], F32)
    sv = [m.view(score, "score%d" % k) for k in range(16)]
    junk = m.sb("junk", [128, S], U8)
    widx = m.sb("widx_s", [128, 8, 16], F32)
    tpos = m.sb("tpos_s", [128, 8], F32)
    slopes = m.sb("slopes_s", [128, 16], F32)
    pow2 = m.sb("pow2_s", [128, NBIS], F32)
    iota = m.sb("iota_s", [128, 512], F32)
    for dst, srcb in ((widx, i_widx), (tpos, E["tpos"]), (slopes, E["slopes"]), (pow2, E["pow2"]), (iota, E["iota"])):
        m.dma("sp", dst[:], srcb[:], reads=[srcb], writes=[dst])
    for r in range(NCORES):
        for cc in range(2):
            m.dma("sp", ckv[:, cc, :].rearrange("p (j r i) -> p j r i", r=8, i=128)[:, :, r, :],
                  g_ckv[r * 256 + cc * 128:r * 256 + (cc + 1) * 128, :].rearrange("p (j i) -> p j i", i=128),
                  reads=[g_ckv], writes=[ckv])
        m.dma("sp", kidx[:, :].rearrange("p (j r i) -> p j r i", r=8, i=128)[:, :, r, :],
              g_kidx[r * 128:(r + 1) * 128, :].rearrange("p (j i) -> p j i", i=128), reads=[g_kidx], writes=[kidx])
    m.op("pool", lambda h: h.memset(vaug[:], 1.0), writes=[vaug])
    for r in range(NCORES):
        m.dma("sp", vaug[:, :, :].rearrange("p (j r) c -> p j r c", r=8)[:, :, r, 0:256],
              g_v[r * T:(r + 1) * T, :].rearrange("(j i) c -> i j c", i=128), reads=[g_v], writes=[vaug])

    A = AttnBufs(m, 256)
    Lb = [m.ps("L%d" % i, [128, 512]) for i in range(2)]
    tmp = [m.sb("tmp%d" % i, [128, 512], F32) for i in range(3)]
    qlat = [m.sb("qlat%d" % i, [128, 2, 16, 128], BF16) for i in range(2)]
    qidx = [m.sb("qidx%d" % i, [128, 16, 128], BF16) for i in range(2)]
    small = {k: m.sb("sm_" + k, [128, 1], F32) for k in ("habs", "lo", "mid", "cnt", "g", "gneg")}
    steps = m.sb("steps", [128, NBIS], F32)
    gm = m.sb("gm", [128, 16], F32)
    G = m.sb("G", [128, 16], F32)
    nL = 0
    nT = 0
    for j in range(NSLOT):
        NK = 2 * j + 2
        N = NK * 512
        ql, qi = qlat[j % 2], qidx[j % 2]
        for cc in range(2):
            m.dma("sp", ql[:, cc, :, :], i_qlat[:, cc, :, j * 128:(j + 1) * 128], reads=[i_qlat], writes=[ql])
        m.dma("sp", qi[:, :, :], i_qidx[:, :, j * 128:(j + 1) * 128], reads=[i_qidx], writes=[qi])
        for kt in range(NK):
            ks = slice(kt * 512, (kt + 1) * 512)
            for hh in range(16):
                L = Lb[nL % 2]
                nL += 1
                mm(m, L, L[:, :], qi, qi[:, hh, :], kidx, kidx[:, ks], True, True)
                if hh == 0:
                    m.op("dve", lambda h, L=L, ks=ks, j=j: h.tensor_scalar(
                        out=score[:, ks], in0=L[:, :], scalar1=0.0, scalar2=widx[:, j, 0:1], op0=ALU.max, op1=ALU.mult),
                        reads=[L, widx], writes=[sv[kt]])
                else:
                    tb = tmp[nT % 3]
                    nT += 1
                    m.op("dve", lambda h, L=L, tb=tb, j=j, hh=hh: h.tensor_scalar(
                        out=tb[:], in0=L[:, :], scalar1=0.0, scalar2=widx[:, j, hh:hh + 1], op0=ALU.max, op1=ALU.mult),
                        reads=[L, widx], writes=[tb])
                    m.op("pool", lambda h, tb=tb, ks=ks: h.tensor_tensor(out=score[:, ks], in0=score[:, ks], in1=tb[:], op=ALU.add),
                         reads=[tb, sv[kt]], writes=[sv[kt]])
        habs, lo, mid, cnt, g, gneg = (small[k] for k in ("habs", "lo", "mid", "cnt", "g", "gneg"))
        m.op("dve", lambda h, N=N: h.tensor_reduce(out=habs[:], in_=score[:, 0:N], axis=AX.X, op=ALU.max, apply_absolute_value=True),
             reads=sv[0:NK], writes=[habs])
        for kt in (2 * j, 2 * j + 1):
            ks = slice(kt * 512, (kt + 1) * 512)
            tb = tmp[nT % 3]
            nT += 1
            m.op("dve", lambda h, tb=tb, j=j, kt=kt: h.tensor_scalar(
                out=tb[:], in0=iota[:], scalar1=tpos[:, j:j + 1], scalar2=float(kt * 512), op0=ALU.subtract, op1=ALU.add),
                reads=[iota, tpos], writes=[tb])
            m.op("dve", lambda h, tb=tb: h.tensor_scalar(out=tb[:], in0=tb[:], scalar1=0.0, scalar2=-1.0e30, op0=ALU.is_gt, op1=ALU.mult),
                 reads=[tb], writes=[tb])
            m.op("dve", lambda h, tb=tb, ks=ks: h.tensor_tensor(out=score[:, ks], in0=score[:, ks], in1=tb[:], op=ALU.add),
                 reads=[tb, sv[kt]], writes=[sv[kt]])
        m.op("dve", lambda h: h.tensor_scalar(out=steps[:], in0=pow2[:], scalar1=habs[:], scalar2=None, op0=ALU.mult),
             reads=[pow2, habs], writes=[steps])
        m.op("dve", lambda h: h.tensor_scalar(out=lo[:], in0=habs[:], scalar1=-1.0, scalar2=None, op0=ALU.mult),
             reads=[habs], writes=[lo])
        for k in range(NBIS):
            m.op("dve", lambda h, k=k: h.tensor_tensor(out=mid[:], in0=lo[:], in1=steps[:, k:k + 1], op=ALU.add),
                 reads=[lo, steps], writes=[mid])
            m.op("dve", lambda h, N=N: h.tensor_scalar(out=junk[:, 0:N], in0=score[:, 0:N], scalar1=mid[:], scalar2=None,
                                                       op0=ALU.is_ge, op1=ALU.add, accum_out=cnt[:]),
                 reads=sv[0:NK] + [mid], writes=[junk, cnt])
            m.op("dve", lambda h, k=k: h.tensor_scalar(out=g[:], in0=cnt[:], scalar1=255.5, scalar2=steps[:, k:k + 1],
                                                       op0=ALU.is_gt, op1=ALU.mult), reads=[cnt, steps], writes=[g])
            m.op("dve", lambda h: h.tensor_tensor(out=lo[:], in0=lo[:], in1=g[:], op=ALU.add), reads=[lo, g], writes=[lo])
        for kt in range(NK):
            ks = slice(kt * 512, (kt + 1) * 512)
            tb = tmp[nT % 3]
            nT += 1
            m.op("dve", lambda h, ks=ks: h.tensor_scalar(out=score[:, ks], in0=score[:, ks], scalar1=lo[:], scalar2=BIGD,
                                                         op0=ALU.is_ge, op1=ALU.mult), reads=[sv[kt], lo], writes=[sv[kt]])
            m.op("dve", lambda h, tb=tb, j=j, kt=kt: h.tensor_scalar(
                out=tb[:], in0=iota[:], scalar1=tpos[:, j:j + 1], scalar2=float(kt * 512) - BIGD, op0=ALU.subtract, op1=ALU.add),
                reads=[iota, tpos], writes=[tb])
            m.op("pool", lambda h, tb=tb, ks=ks: h.tensor_tensor(out=score[:, ks], in0=score[:, ks], in1=tb[:], op=ALU.add),
                 reads=[tb, sv[kt]], writes=[sv[kt]])
            m.op("dve", lambda h, ks=ks, kt=kt: h.tensor_reduce(out=gm[:, kt:kt + 1], in_=score[:, ks], axis=AX.X, op=ALU.max),
                 reads=[sv[kt]], writes=[gm])
        m.op("dve", lambda h, NK=NK: h.tensor_reduce(out=gneg[:], in_=gm[:, 0:NK], axis=AX.X, op=ALU.max), reads=[gm], writes=[gneg])
        m.op("dve", lambda h: h.tensor_scalar(out=G[:], in0=slopes[:], scalar1=gneg[:], scalar2=-1.0, op0=ALU.mult, op1=ALU.mult),
             reads=[slopes, gneg], writes=[G])
        for hh in range(16):
            acc = A.acc[hh % 2]
            for kt in range(NK):
                ks = slice(kt * 512, (kt + 1) * 512)

                def score_fn(Sb, hh=hh, ks=ks):
                    mm(m, Sb, Sb[:, :], ql, ql[:, 0, hh, :], ckv, ckv[:, 0, ks], True, False)
                    mm(m, Sb, Sb[:, :], ql, ql[:, 1, hh, :], ckv, ckv[:, 1, ks], False, True)

                def exp_fn(sc, p, hh=hh):
                    m.op("act", lambda h: h.activation(out=p[:], in_=sc[:], func=AF.Exp, bias=G[:, hh:hh + 1], scale=1.0),
                         reads=[sc, G], writes=[p])

                attn_tile(m, A, acc, score_fn, sv[kt], score[:, ks], SLOPES[hh], exp_fn, vaug,
                          [vaug[:, kt * 4 + q, :] for q in range(4)], kt == 0, kt == NK - 1)
            attn_finish(m, A, acc, oT, hh, j)
    m.phase_end()


def emit_Q(m, E, dsa, j, l, xT, xoT, modsb, SC):
    m.phase_begin()
    NO = 32 if dsa else 16
    i_o = SC["oT_dsa"] if dsa else SC["oT_moba"]
    i_sg = SC["sgT"]
    w_o = E[("dsa_w_o%d" if dsa else "moba_w_o%d") % j]
    lin = LinT(m, 16)
    onesf = m.sb("onesf", [128, 128], F32)
    m.op("pool", lambda h: h.memset(onesf[:], 1.0), writes=[onesf])
    gp = m.sb("gp", [128, 16], F32)
    lng = m.sb("lng", [128, 16], F32)
    lnb = m.sb("lnb", [128, 16], F32)
    epsb = m.sb("epsb", [128, 1], F32)
    m.op("dve", lambda h: h.memset(epsb[:], LN_EPS / (ALPHA * ALPHA)), writes=[epsb])
    m.dma("sp", lng[:], E["lngT"][:, l, :], reads=[E["lngT"]], writes=[lng])
    m.dma("sp", lnb[:], E["lnbT"][:, l, :], reads=[E["lnbT"]], writes=[lnb])
    m.op("dve", lambda h: h.tensor_scalar(out=gp[:], in0=modsb[:, l, 32:48], scalar1=1.0 / ALPHA, scalar2=None, op0=ALU.mult),
         reads=[modsb], writes=[gp])
    if dsa:
        i_wuv = E["dsa_w_uv%d" % j]
        wuv = m.sb("wuv", [128, 16, 2, 128], BF16)
        for cc in range(2):
            m.dma("pool", wuv[:, :, cc, :], i_wuv[:, cc * 128:(cc + 1) * 128, :].rearrange("h p d -> p h d"),
                  reads=[i_wuv], writes=[wuv])
    obf = m.sb("obf", [128, NO, 512], BF16)
    sg = m.sb("sg", [128, 16, 512], BF16)
    og = m.sb("og", [128, 16, 512], BF16)
    z = m.sb("z", [128, 16, 512], F32)
    pso = [m.ps("pso%d" % i, [128, 512]) for i in range(2)]
    pstat = m.ps("pstat", [128, 512])
    mean = m.sb("mean", [128, 512], F32)
    rstd = m.sb("rstd", [128, 512], F32)
    sq = [m.sb("sq%d" % i, [128, 512], F32) for i in range(2)]
    for t0 in range(0, T, 512):
        ts = slice(t0, t0 + 512)
        step = NO // 2
        for a in range(0, NO, step):
            m.dma("sp", obf[:, a:a + step, :], i_o[:, a:a + step, ts], reads=[i_o], writes=[obf])
        m.dma("sp", sg[:, :, :], i_sg[:, :, ts], reads=[i_sg], writes=[sg])
        for a in range(0, 16, 8):
            m.dma("sp", z[:, a:a + 8, :], xT[:, a:a + 8, ts], reads=[xT], writes=[z])
        for hh in range(16):
            if dsa:
                ps = pso[hh % 2]
                for cc in range(2):
                    mm(m, ps, ps[:, :], wuv, wuv[:, hh, cc, :], obf, obf[:, hh * 2 + cc, :], cc == 0, cc == 1)
                m.op("dve", lambda h, ps=ps, hh=hh: h.tensor_tensor(out=og[:, hh, :], in0=ps[:, :], in1=sg[:, hh, :], op=ALU.mult),
                     reads=[ps, sg], writes=[og])
            else:
                m.op("dve", lambda h, hh=hh: h.tensor_tensor(out=og[:, hh, :], in0=obf[:, hh, :], in1=sg[:, hh, :], op=ALU.mult),
                     reads=[obf, sg], writes=[og])

        def ev_y(fa, mr, tt0, tn, ps, ps_ap):
            kc = fa // 128
            m.op("dve", lambda h: h.scalar_tensor_tensor(out=z[:, kc, :], in0=ps_ap, scalar=gp[:, kc:kc + 1], in1=z[:, kc, :],
                                                         op0=ALU.mult, op1=ALU.add), reads=[ps, gp, z], writes=[z])

        lin.run(w_o, w_o, 0, 2048, 16, og, 512, ev_y)
        for kc in range(16):
            mm(m, pstat, pstat[:, :], onesf, onesf[:, :], z, z[:, kc, :], kc == 0, kc == 15)
        m.op("act", lambda h: h.activation(out=mean[:], in_=pstat[:, :], func=AF.Copy, scale=1.0 / D), reads=[pstat], writes=[mean])
        for kc in range(16):
            m.op("dve", lambda h, kc=kc: h.tensor_tensor(out=z[:, kc, :], in0=z[:, kc, :], in1=mean[:], op=ALU.subtract),
                 reads=[z, mean], writes=[z])
        for kc in range(16):
            s = sq[kc % 2]
            m.op("act", lambda h, s=s, kc=kc: h.activation(out=s[:], in_=z[:, kc, :], func=AF.Square), reads=[z], writes=[s])
            mm(m, pstat, pstat[:, :], onesf, onesf[:, :], s, s[:, :], kc == 0, kc == 15)
        m.op("act", lambda h: h.activation(out=rstd[:], in_=pstat[:, :], func=AF.Sqrt, bias=epsb[:], scale=1.0 / D),
             reads=[pstat, epsb], writes=[rstd])
        m.op("dve", lambda h: h.reciprocal(out=rstd[:], in_=rstd[:]), reads=[rstd], writes=[rstd])
        for kc in range(16):
            s = sq[kc % 2]
            m.op("dve", lambda h, s=s, kc=kc: h.scalar_tensor_tensor(out=s[:], in0=z[:, kc, :], scalar=lng[:, kc:kc + 1], in1=rstd[:],
                                                                     op0=ALU.mult, op1=ALU.mult), reads=[z, lng, rstd], writes=[s])
            m.op("act", lambda h, s=s, kc=kc: h.activation(out=z[:, kc, :], in_=s[:], func=AF.Identity, bias=lnb[:, kc:kc + 1], scale=1.0),
                 reads=[s, lnb, z], writes=[z])
        for a in range(0, 16, 8):
            m.dma("sp", xoT[:, a:a + 8, ts], z[:, a:a + 8, :], reads=[z], writes=[xoT])
    m.phase_end()


def emit_P_moba(m, E, j, l, xT, modsb, SC):
    m.phase_begin()
    w_in = E["moba_w_in%d" % j]
    o_q, s_k, s_v, o_sg = SC["qT"], SC["s_k"], SC["s_vm"], SC["sgT"]
    hT = m.sb("hT", [128, 16, T], BF16)
    modulate(m, xT, modsb, l, hT, T)
    lin = LinT(m, 16)
    st = Stager(m)

    def ev_q(fa, mr, t0, tn, ps, ps_ap):
        st.put(o_q, o_q[:, fa // 128, t0:t0 + tn], ps, ps_ap, mr, "act" if (fa // 128) % 2 else "dve", scale=SCALE)

    def ev_k(fa, mr, t0, tn, ps, ps_ap):
        st.put(s_k, s_k[fa - 2048:fa - 2048 + mr, t0:t0 + tn], ps, ps_ap, mr, "act" if (fa // 128) % 2 else "dve")

    def ev_g(fa, mr, t0, tn, ps, ps_ap):
        st.put(o_sg, o_sg[:, (fa - 6144) // 128, t0:t0 + tn], ps, ps_ap, mr, "act", func=AF.Silu)

    lin.run(w_in, w_in, 2048, 2048, 16, hT, T, ev_k)
    m.allgather(s_k, SC["g_k"])
    nb = 0
    sv_view = s_v.t.rearrange("(j i) f -> i j f", i=128)
    for fb in range(4):
        slot = lin.load(w_in, w_in, 4096 + fb * 512, 512, 16)
        for jj in range(8):
            ps = lin.banks[nb % 2]
            nb += 1
            for kc in range(16):
                mm(m, ps, ps[:, :], hT, hT[:, kc, jj * 128:(jj + 1) * 128], slot, slot[:, kc, :], kc == 0, kc == 15)
            st.put(s_v, sv_view[:, jj, fb * 512:(fb + 1) * 512], ps, ps[:, :], 128, "act" if nb % 2 else "dve")
    m.allgather(s_v, SC["g_vm"])
    lin.run(w_in, w_in, 0, 2048, 16, hT, T, ev_q)
    lin.run(w_in, w_in, 6144, 2048, 16, hT, T, ev_g)
    m.phase_end()


def emit_B_moba(m, E, SC):
    m.phase_begin()
    i_q, g_k, g_v = SC["qT"], SC["g_k"], SC["g_vm"]
    oT = SC["oT_moba"]
    tpos = m.sb("tpos_s", [128, 8], F32)
    own = m.sb("own_s", [128, 8], F32)
    iota = m.sb("iota_s", [128, 512], F32)
    blkend = m.sb("blkend_s", [128, 32], F32)
    s0row = m.sb("s0row_s", [128, 8, 32], F32)
    for dst, srcb in ((tpos, E["tpos"]), (own, E["ownstart"]), (iota, E["iota"]), (blkend, E["blkend"]), (s0row, E["s0row"])):
        m.dma("sp", dst[:], srcb[:], reads=[srcb], writes=[dst])
    dpast = m.sb("dpast", [128, 8, 512], F32)
    ddiag = m.sb("ddiag", [128, 8, 2, 512], F32)
    pastpen = m.sb("pastpen", [128, 8, 32], F32)
    pastf = m.sb("pastf", [128, 8, 32], F32)
    tcz = m.sb("tcz", [128, 512], F32)
    for j in range(8):
        m.op("dve", lambda h, j=j: h.tensor_scalar(out=dpast[:, j, :], in0=iota[:], scalar1=tpos[:, j:j + 1], scalar2=None,
                                                   op0=ALU.subtract), reads=[iota, tpos], writes=[dpast])
        for u in range(2):
            kt = 2 * j + u
            m.op("dve", lambda h, j=j, u=u, kt=kt: h.tensor_scalar(out=ddiag[:, j, u, :], in0=iota[:], scalar1=tpos[:, j:j + 1],
                                                                  scalar2=float(kt * 512), op0=ALU.subtract, op1=ALU.add),
                 reads=[iota, tpos], writes=[ddiag])
            m.op("dve", lambda h, j=j, u=u: h.tensor_scalar(out=tcz[:], in0=ddiag[:, j, u, :], scalar1=0.0, scalar2=-BIGD,
                                                            op0=ALU.is_gt, op1=ALU.mult), reads=[ddiag], writes=[tcz])
            m.op("dve", lambda h, j=j, u=u: h.tensor_tensor(out=ddiag[:, j, u, :], in0=ddiag[:, j, u, :], in1=tcz[:], op=ALU.add),
                 reads=[ddiag, tcz], writes=[ddiag])
        m.op("dve", lambda h, j=j: h.tensor_scalar(out=pastpen[:, j, :], in0=blkend[:], scalar1=own[:, j:j + 1], scalar2=-1.0e30,
                                                   op0=ALU.is_ge, op1=ALU.mult), reads=[blkend, own], writes=[pastpen])
        m.op("dve", lambda h, j=j: h.tensor_scalar(out=pastf[:, j, :], in0=blkend[:], scalar1=own[:, j:j + 1], scalar2=30000.0,
                                                   op0=ALU.is_lt, op1=ALU.mult), reads=[blkend, own], writes=[pastf])
    A = AttnBufs(m, 128)
    kh = [m.sb("kh%d" % i, [128, S], BF16) for i in range(2)]
    vh = [m.sb("vh%d" % i, [128, 64, 129], BF16) for i in range(2)]
    qh = [m.sb("qh%d" % i, [128, T], BF16) for i in range(2)]
    for v in vh:
        m.op("pool", lambda h, v=v: h.memset(v[:], 1.0), writes=[v])
    km = m.sb("km", [128, 32], F32)
    kmh = m.sb("kmh", [128, 32], BF16)
    kml = m.sb("kml", [128, 32], BF16)
    gps = [m.ps("gps%d" % i, [128, 512]) for i in range(2)]
    gp = m.sb("gp", [128, 32], F32)
    ga = m.sb("ga", [128, 32], F32)
    t1 = m.sb("t1", [128, 32], F32)
    mx = m.sb("mx", [128, 1], F32)
    BT = [m.sb("BT%d" % i, [128, 32], F32) for i in range(2)]
    ng = 0
    for hh in range(16):
        K_h, V_h, Q_h = kh[hh % 2], vh[hh % 2], qh[hh % 2]
        for r in range(NCORES):
            m.dma("sp", K_h[:, :].rearrange("p (j r i) -> p j r i", r=8, i=128)[:, :, r, :],
                  g_k[r * 2048 + hh * 128:r * 2048 + (hh + 1) * 128, :].rearrange("p (j i) -> p j i", i=128),
                  reads=[g_k], writes=[K_h])
            m.dma("sp", V_h[:, :, :].rearrange("p (j r) c -> p j r c", r=8)[:, :, r, 0:128],
                  g_v[r * T:(r + 1) * T, hh * 128:(hh + 1) * 128].rearrange("(j i) c -> i j c", i=128),
                  reads=[g_v], writes=[V_h])
        m.dma("sp", Q_h[:, :], i_q[:, hh, :], reads=[i_q], writes=[Q_h])
        m.op("dve", lambda h, K_h=K_h: h.tensor_reduce(out=km[:], in_=K_h[:, :].rearrange("p (n s) -> p n s", s=256), axis=AX.X, op=ALU.add),
             reads=[K_h], writes=[km])
        m.op("dve", lambda h: h.tensor_scalar(out=km[:], in0=km[:], scalar1=1.0 / 256, scalar2=None, op0=ALU.mult), reads=[km], writes=[km])
        m.op("dve", lambda h: h.tensor_copy(out=kmh[:], in_=km[:]), reads=[km], writes=[kmh])
        m.op("dve", lambda h: h.tensor_tensor(out=kml[:], in0=km[:], in1=kmh[:], op=ALU.subtract), reads=[km, kmh], writes=[kml])
        for j in range(NSLOT):
            NK = 2 * j + 2
            qs = slice(j * 128, (j + 1) * 128)
            g_ps = gps[ng % 2]
            bt = BT[ng % 2]
            ng += 1
            mm(m, g_ps, g_ps[:, 0:32], Q_h, Q_h[:, qs], kmh, kmh[:], True, False)
            mm(m, g_ps, g_ps[:, 0:32], Q_h, Q_h[:, qs], kml, kml[:], False, True)
            m.op("dve", lambda h, g_ps=g_ps, j=j: h.tensor_tensor(out=gp[:], in0=g_ps[:, 0:32], in1=pastpen[:, j, :], op=ALU.add),
                 reads=[g_ps, pastpen], writes=[gp])
            srcb = gp
            for r in range(2):
                m.op("dve", lambda h, srcb=srcb: h.tensor_reduce(out=mx[:], in_=srcb[:], axis=AX.X, op=ALU.max), reads=[srcb], writes=[mx])
                m.op("dve", lambda h, srcb=srcb: h.tensor_scalar(out=t1[:], in0=srcb[:], scalar1=mx[:], scalar2=-1.0e30, op0=ALU.is_ge, op1=ALU.mult),
                     reads=[srcb, mx], writes=[t1])
                m.op("dve", lambda h, srcb=srcb: h.tensor_tensor(out=ga[:], in0=srcb[:], in1=t1[:], op=ALU.add), reads=[srcb, t1], writes=[ga])
                srcb = ga
            m.op("dve", lambda h: h.tensor_reduce(out=mx[:], in_=ga[:], axis=AX.X, op=ALU.max), reads=[ga], writes=[mx])
            m.op("dve", lambda h: h.tensor_scalar(out=t1[:], in0=gp[:], scalar1=mx[:], scalar2=1.0, op0=ALU.is_ge, op1=ALU.subtract),
                 reads=[gp, mx], writes=[t1])
            m.op("dve", lambda h, j=j: h.tensor_tensor(out=t1[:], in0=t1[:], in1=pastf[:, j, :], op=ALU.mult), reads=[t1, pastf], writes=[t1])
            m.op("dve", lambda h, j=j, bt=bt, hh=hh: h.scalar_tensor_tensor(out=bt[:], in0=s0row[:, j, :], scalar=SLOPES[hh], in1=t1[:],
                                                                           op0=ALU.mult, op1=ALU.add), reads=[s0row, t1], writes=[bt])
            acc = A.acc[ng % 2]
            for kt in range(NK):
                ks = slice(kt * 512, (kt + 1) * 512)
                if kt < 2 * j:
                    dm_b, dm_ap = dpast, dpast[:, j, :]
                else:
                    dm_b, dm_ap = ddiag, ddiag[:, j, kt - 2 * j, :]

                def score_fn(Sb, ks=ks, qs=qs, Q_h=Q_h, K_h=K_h):
                    mm(m, Sb, Sb[:, :], Q_h, Q_h[:, qs], K_h, K_h[:, ks], True, True)

                def exp_fn(sc, p, kt=kt, bt=bt):
                    for u in range(2):
                        m.op("act", lambda h, u=u: h.activation(out=p[:, u * 256:(u + 1) * 256], in_=sc[:, u * 256:(u + 1) * 256], func=AF.Exp,
                                                                bias=bt[:, 2 * kt + u:2 * kt + u + 1], scale=1.0),
                             reads=[sc, bt], writes=[p])

                attn_tile(m, A, acc, score_fn, dm_b, dm_ap, SLOPES[hh], exp_fn, V_h,
                          [V_h[:, kt * 4 + q, :] for q in range(4)], kt == 0, kt == NK - 1)
            attn_finish(m, A, acc, oT, hh, j)
    m.phase_end()


def build_all(nlayers=DEPTH, layers=None):
    nc = bass.Bass("TRN2", target_bir_lowering=False)
    m = MK(nc)
    E = {}
    E["xT"] = m.din("xT", [128, 16, T], F32)
    E["cT"] = m.din("cT", [128, 16], F32)
    E["aw"] = m.din("aw", [24, 128, 16, 128], F32)
    E["ab"] = m.din("ab", [128, 24], F32)
    E["lngT"] = m.din("lngT", [128, DEPTH, 16], F32)
    E["lnbT"] = m.din("lnbT", [128, DEPTH, 16], F32)
    for j in range(2):
        E["dsa_w_in%d" % j] = m.din("dsa_w_in%d" % j, [2048, 2960], F32)
        E["dsa_gqT%d" % j] = m.din("dsa_gqT%d" % j, [128, 4], F32)
        E["dsa_gkvT%d" % j] = m.din("dsa_gkvT%d" % j, [128, 2], F32)
        E["dsa_w_uq%d" % j] = m.din("dsa_w_uq%d" % j, [512, 2048], F32)
        E["dsa_w_qi%d" % j] = m.din("dsa_w_qi%d" % j, [512, 2048], F32)
        E["dsa_w_uk%d" % j] = m.din("dsa_w_uk%d" % j, [16, 128, 256], F32)
        E["dsa_w_uv%d" % j] = m.din("dsa_w_uv%d" % j, [16, 256, 128], F32)
        E["dsa_w_o%d" % j] = m.din("dsa_w_o%d" % j, [2048, 2048], F32)
        E["moba_w_in%d" % j] = m.din("moba_w_in%d" % j, [2048, 8192], F32)
        E["moba_w_o%d" % j] = m.din("moba_w_o%d" % j, [2048, 2048], F32)
    E["tpos"] = m.din("tpos", [128, 8], F32)
    E["ownstart"] = m.din("ownstart", [128, 8], F32)
    E["slopes"] = m.din("slopes", [128, 16], F32)
    E["pow2"] = m.din("pow2", [128, NBIS], F32)
    E["iota"] = m.din("iota", [128, 512], F32)
    E["blkend"] = m.din("blkend", [128, 32], F32)
    E["s0row"] = m.din("s0row", [128, 8, 32], F32)
    out = m.dout("xoT", [128, 16, T], F32)
    SC = {
        "mod_s": m.dram("mod_s", [128, 24], F32), "mod_g": m.dram("mod_g", [1024, 24], F32),
        "xa": m.dram("xa", [128, 16, T], F32), "xb": m.dram("xb", [128, 16, T], F32),
        "qlatT": m.dram("qlatT", [128, 2, 16, T], BF16), "qidxT": m.dram("qidxT", [128, 16, T], BF16),
        "widx": m.dram("widx", [128, 8, 16], F32), "sgT": m.dram("sgT", [128, 16, T], BF16),
        "s_ckv": m.dram("s_ckv", [256, T], BF16), "g_ckv": m.dram("g_ckv", [2048, T], BF16),
        "s_kidx": m.dram("s_kidx", [128, T], BF16), "g_kidx": m.dram("g_kidx", [1024, T], BF16),
        "s_v": m.dram("s_v", [T, 256], BF16), "g_v": m.dram("g_v", [S, 256], BF16),
        "oT_dsa": m.dram("oT_dsa", [128, 32, T], BF16), "oT_moba": m.dram("oT_moba", [128, 16, T], BF16),
        "qT": m.dram("qT", [128, 16, T], BF16),
        "s_k": m.dram("s_k", [2048, T], BF16), "g_k": m.dram("g_k", [8 * 2048, T], BF16),
        "s_vm": m.dram("s_vm", [T, 2048], BF16), "g_vm": m.dram("g_vm", [S, 2048], BF16),
    }
    modsb = m.sb("modsb", [128, DEPTH, 48], F32)
    emit_M(m, E, modsb, SC)
    xin = E["xT"]
    layers = list(range(nlayers)) if layers is None else layers
    for l in layers:
        j = l // 2
        xout = out if l == layers[-1] else (SC["xa"] if l % 2 == 0 else SC["xb"])
        if l % 2 == 0:
            emit_P_dsa(m, E, j, l, xin, modsb, SC)
            emit_B_dsa(m, E, SC)
            emit_Q(m, E, True, j, l, xin, xout, modsb, SC)
        else:
            emit_P_moba(m, E, j, l, xin, modsb, SC)
            emit_B_moba(m, E, SC)
            emit_Q(m, E, False, j, l, xin, xout, modsb, SC)
        xin = xout
    m.finish()
    return nc


_PROGS = {}


def prog(name, builder):
    if name not in _PROGS:
        _PROGS[name] = builder()
    return _PROGS[name]


def fm(a):
    F, Tn = a.shape
    return np.ascontiguousarray(a.reshape(F // 128, 128, Tn).transpose(1, 0, 2))


def unfm(a):
    P, KC, Tn = a.shape
    return np.ascontiguousarray(a.transpose(1, 0, 2).reshape(KC * 128, Tn))


def vecT(v):
    return np.ascontiguousarray(v.reshape(-1, 128).T)


def core_positions(core):
    j = np.arange(8)[:, None]
    i = np.arange(128)[None, :]
    return ((8 * j + core) * 128 + i).reshape(-1)


def kernel(x, c, ada_w, ada_b, ln_g, ln_b, dsa_w_in, dsa_g_q, dsa_g_kv, dsa_w_uq, dsa_w_qi,
           dsa_w_uk, dsa_w_uv, dsa_w_o, moba_w_in, moba_w_o, _nlayers=DEPTH):
    f32 = lambda a: np.ascontiguousarray(np.asarray(a, dtype=np.float32))
    x, c, ada_w, ada_b, ln_g, ln_b = map(f32, (x, c, ada_w, ada_b, ln_g, ln_b))
    dsa_w_in, dsa_g_q, dsa_g_kv, dsa_w_uq, dsa_w_qi = map(f32, (dsa_w_in, dsa_g_q, dsa_g_kv, dsa_w_uq, dsa_w_qi))
    dsa_w_uk, dsa_w_uv, dsa_w_o, moba_w_in, moba_w_o = map(f32, (dsa_w_uk, dsa_w_uv, dsa_w_o, moba_w_in, moba_w_o))
    nc = prog("all%d" % _nlayers, lambda: build_all(_nlayers))
    x0 = x[0]
    common = {
        "cT": vecT(c[0]),
        "lngT": np.ascontiguousarray(ln_g.reshape(DEPTH, 16, 128).transpose(2, 0, 1)),
        "lnbT": np.ascontiguousarray(ln_b.reshape(DEPTH, 16, 128).transpose(2, 0, 1)),
        "slopes": np.tile(np.asarray(SLOPES, np.float32)[None, :], (128, 1)),
        "pow2": np.tile((2.0 ** -np.arange(NBIS, dtype=np.float64)).astype(np.float32)[None, :], (128, 1)),
        "iota": np.tile(np.arange(512, dtype=np.float32)[None, :], (128, 1)),
        "blkend": np.tile((np.arange(32, dtype=np.float32) * 256 + 255)[None, :], (128, 1)),
    }
    s0row = np.zeros((128, 8, 32), np.float32)
    for j in range(8):
        for n in range(4 * j):
            s0row[:, j, n] = 512.0 * (n // 2)
    common["s0row"] = s0row
    for j in range(2):
        common["dsa_w_in%d" % j] = dsa_w_in[j]
        common["dsa_gqT%d" % j] = vecT(dsa_g_q[j])
        common["dsa_gkvT%d" % j] = vecT(dsa_g_kv[j])
        common["dsa_w_uq%d" % j] = dsa_w_uq[j]
        common["dsa_w_qi%d" % j] = dsa_w_qi[j]
        common["dsa_w_uk%d" % j] = dsa_w_uk[j]
        common["dsa_w_uv%d" % j] = dsa_w_uv[j]
        common["dsa_w_o%d" % j] = dsa_w_o[j]
        common["moba_w_in%d" % j] = moba_w_in[j]
        common["moba_w_o%d" % j] = moba_w_o[j]
    in_maps = []
    for core in range(NCORES):
        d = dict(common)
        pos = core_positions(core)
        d["xT"] = fm(np.ascontiguousarray(x0[pos].T))
        aw = np.empty((24, 128, 16, 128), np.float32)
        ab = np.empty((128, 24), np.float32)
        for l in range(DEPTH):
            for q in range(6):
                f0 = (core * 6 + q) * 128
                aw[l * 6 + q] = ada_w[l][:, f0:f0 + 128].reshape(16, 128, 128).transpose(1, 0, 2)
                ab[:, l * 6 + q] = ada_b[l][f0:f0 + 128]
        d["aw"], d["ab"] = aw, ab
        tp = pos.reshape(8, 128).T.astype(np.float32)
        d["tpos"] = np.ascontiguousarray(tp)
        d["ownstart"] = np.ascontiguousarray(np.floor(tp / 256.0).astype(np.float32) * 256.0)
        in_maps.append(d)
    res = run_bass_kernel_spmd(nc, in_maps, core_ids=list(range(NCORES))).results
    out = np.empty((1, S, D), np.float32)
    for core in range(NCORES):
        out[0, core_positions(core)] = unfm(res[core]["xoT"]).T
    return out
```

```python
import numpy as np
import ml_dtypes
import concourse.bass as bass
import concourse.mybir as mybir
from concourse.bass_utils import run_bass_kernel_spmd

F32 = mybir.dt.float32
BF16 = mybir.dt.bfloat16
U8 = mybir.dt.uint8
ALU = mybir.AluOpType
AF = mybir.ActivationFunctionType
AX = mybir.AxisListType
NPBF = ml_dtypes.bfloat16

NCORES = 8
S = 8192
D = 2048
T = 1024
NSLOT = 8
H = 16
DEPTH = 4
ALPHA = float((2 * DEPTH) ** 0.25)
LN_EPS = 1e-5
SCALE = 128 ** -0.5
SLOPES = [float(2.0 ** (-8.0 * (h + 1) / 16)) for h in range(16)]
BIGD = 1.0e6
NBIS = 24


class Buf:
    def __init__(self, name, t, kind):
        self.name = name
        self.t = t
        self.kind = kind
        self.wev = {}
        self.rev = {}
        self.dsems = {}

    def __getitem__(self, idx):
        return self.t[idx]


class MK:
    ENG = ("pe", "act", "dve", "pool", "sp")

    def __init__(self, nc, self_sync=True):
        self.nc = nc
        self.self_sync = self_sync
        self.streams = {e: [] for e in self.ENG}
        self.sems = {}
        self.count = {e: 0 for e in self.ENG}
        self.semcount = {}
        self.seen = {e: {} for e in self.ENG}
        self.ctx = []
        self.pctx = []
        self.in_phase = False
        self.free_dsem = {"sp": [], "pool": [], "act": []}
        self.holders = []
        self.ndsem = 0
        self.ncc = 0
        self.outs = []
        for e in ("pe", "act", "dve", "pool"):
            self._mksem("E_" + e)
        self._mksem("CC")
        self.ccdummy = self.sb("ccdummy", [128, 8], F32)

    def _mksem(self, key):
        cm = self.nc.semaphore(key)
        h = cm.__enter__()
        self.ctx.append(cm)
        self.sems[key] = h
        self.semcount[key] = 0
        return key

    def _enter(self, cm):
        t = cm.__enter__()
        (self.pctx if self.in_phase else self.ctx).append(cm)
        return t

    def _uname(self, name):
        self.uid = getattr(self, "uid", 0) + 1
        return "%s_u%d" % (name, self.uid)

    def sb(self, name, shape, dtype):
        return Buf(name, self._enter(self.nc.sbuf_tensor(self._uname(name), list(shape), dtype)), "sb")

    def ps(self, name, shape, dtype=F32):
        return Buf(name, self._enter(self.nc.psum_tensor(self._uname(name), list(shape), dtype)), "ps")

    def dram(self, name, shape, dtype, kind=None):
        if kind is None:
            t = self.nc.dram_tensor(name, list(shape), dtype)
        else:
            t = self.nc.dram_tensor(name, list(shape), dtype, kind=kind)
        b = Buf(name, t.ap(), "dram")
        if kind == "ExternalOutput":
            self.outs.append(b)
        return b

    def din(self, name, shape, dtype):
        return self.dram(name, shape, dtype, kind="ExternalInput")

    def dout(self, name, shape, dtype):
        return self.dram(name, shape, dtype, kind="ExternalOutput")

    def view(self, buf, name):
        return Buf(name, buf.t, buf.kind)

    def _waits(self, eng, reads, writes, skip_key=None):
        need = {}
        for b in reads:
            for k, v in b.wev.items():
                need[k] = max(need.get(k, 0), v)
        for b in writes:
            for k, v in b.wev.items():
                if k != skip_key:
                    need[k] = max(need.get(k, 0), v)
            for k, v in b.rev.items():
                need[k] = max(need.get(k, 0), v)
        out = []
        seen = self.seen[eng]
        for k, v in need.items():
            if k == "E_" + eng and (eng == "pe" or not self.self_sync):
                continue
            if seen.get(k, 0) >= v:
                continue
            seen[k] = v
            out.append((k, v))
        return out

    def _commit(self, ev, reads, writes):
        k, v = ev
        for b in reads:
            b.rev[k] = max(b.rev.get(k, 0), v)
        for b in writes:
            b.wev = {k: v}
            b.rev = {}

    def op(self, eng, fn, reads=(), writes=()):
        waits = self._waits(eng, reads, writes)
        self.count[eng] += 1
        k = "E_" + eng
        self.semcount[k] = self.count[eng]
        self.streams[eng].append((waits, fn, k, 1))
        self._commit((k, self.count[eng]), reads, writes)

    def dma(self, eng, out_ap, in_ap, reads=(), writes=(), **kw):
        tgt = writes[0]
        key = tgt.dsems.get(eng)
        if key is None:
            if self.free_dsem[eng]:
                key = self.free_dsem[eng].pop()
            else:
                self.ndsem += 1
                key = self._mksem("D%d_%s" % (self.ndsem, eng))
            tgt.dsems[eng] = key
            self.holders.append(tgt)
        waits = self._waits(eng, reads, writes, skip_key=key)
        self.semcount[key] += 16
        ev = (key, self.semcount[key])
        fn = lambda h, o=out_ap, i=in_ap, kw=kw: h.dma_start(out=o, in_=i, **kw)
        self.streams[eng].append((waits, fn, key, 16))
        self._commit(ev, reads, writes)

    def allgather(self, src, dst):
        waits = self._waits("pool", [src], [dst])
        self.ncc += 1
        self.semcount["CC"] = self.ncc

        def cc(h):
            return h.collective_compute("AllGather", ALU.bypass, replica_groups=[list(range(NCORES))],
                                        ins=[src.t.opt()], outs=[dst.t.opt()])
        self.streams["pool"].append((waits, cc, "CC", 1))
        self.seen["pool"]["CC"] = self.ncc
        self.count["pool"] += 1
        self.semcount["E_pool"] = self.count["pool"]
        dummy = self.ccdummy
        self.streams["pool"].append(([("CC", self.ncc)], lambda h: h.memset(dummy[:], 0.0), "E_pool", 1))
        self._commit(("E_pool", self.count["pool"]), [src], [dst])

    def barrier(self):
        for e in self.ENG:
            waits = []
            seen = self.seen[e]
            for k, v in self.semcount.items():
                if k != "CC" and v > 0 and seen.get(k, 0) < v:
                    seen[k] = v
                    waits.append((k, v))
            self.streams[e].append((waits, None, None, 0))

    def phase_begin(self):
        self.in_phase = True

    def phase_end(self):
        self.barrier()
        for cm in reversed(self.pctx):
            cm.__exit__(None, None, None)
        self.pctx = []
        for b in self.holders:
            for eng, key in b.dsems.items():
                self.free_dsem[eng].append(key)
            b.dsems = {}
        self.holders = []
        self.in_phase = False

    def finish(self):
        waits = self._waits("sp", self.outs, ())
        self.streams["sp"].append((waits, None, None, 0))
        nc = self.nc
        sems = self.sems
        streams = self.streams

        def replay(h, items, inline=False):
            for waits, fn, sk, amt in items:
                if fn is None:
                    for k, v in waits:
                        h.wait_ge(sems[k], v)
                    continue
                if inline and waits:
                    for k, v in waits[:-1]:
                        h.wait_ge(sems[k], v)
                    k, v = waits[-1]
                    fn(h)._wait_ge(sems[k], v).then_inc(sems[sk], amt)
                else:
                    for k, v in waits:
                        h.wait_ge(sems[k], v)
                    fn(h).then_inc(sems[sk], amt)

        with nc.Block() as block:
            @block.tensor
            def _(h):
                replay(h, streams["pe"], inline=True)

            @block.scalar
            def _(h):
                replay(h, streams["act"])

            @block.vector
            def _(h):
                replay(h, streams["dve"])

            @block.gpsimd
            def _(h):
                replay(h, streams["pool"])

            @block.sync
            def _(h):
                replay(h, streams["sp"])
        for cm in reversed(self.pctx + self.ctx):
            cm.__exit__(None, None, None)
        self.ctx = []
        self.pctx = []


def mm(m, out_b, out_ap, lhsT_b, lhsT_ap, rhs_b, rhs_ap, start, stop):
    m.op("pe", lambda h: h.matmul(out_ap, lhsT=lhsT_ap, rhs=rhs_ap, start=start, stop=stop),
         reads=[lhsT_b, rhs_b], writes=[out_b])


class LinT:
    def __init__(self, m, KCmax, name="lw"):
        self.m = m
        self.slots = [m.sb("%s%d" % (name, i), [128, KCmax, 512], BF16) for i in range(2)]
        self.banks = [m.ps("%sps%d" % (name, i), [128, 512]) for i in range(2)]
        self.nslot = 0
        self.nbank = 0

    def load(self, W, w_ap, f0, nf, KC):
        m = self.m
        slot = self.slots[self.nslot % 2]
        self.nslot += 1
        src = w_ap[:, f0:f0 + nf].rearrange("(kc p) f -> p kc f", p=128)
        half = max(1, KC // 2)
        for a in range(0, KC, half):
            b = min(KC, a + half)
            m.dma("pool", slot[:, a:b, 0:nf], src[:, a:b, :], reads=[W], writes=[slot])
        return slot

    def run(self, W, w_ap, f0, nf, KC, inT, Tn, evac):
        m = self.m
        for fb in range(f0, f0 + nf, 512):
            nb = min(512, f0 + nf - fb)
            slot = self.load(W, w_ap, fb, nb, KC)
            for fs in range(0, nb, 128):
                mr = min(128, nb - fs)
                for t0 in range(0, Tn, 512):
                    tn = min(512, Tn - t0)
                    ps = self.banks[self.nbank % 2]
                    self.nbank += 1
                    for kc in range(KC):
                        mm(m, ps, ps[0:mr, 0:tn], slot, slot[:, kc, fs:fs + mr],
                           inT, inT[:, kc, t0:t0 + tn], kc == 0, kc == KC - 1)
                    evac(fb + fs, mr, t0, tn, ps, ps[0:mr, 0:tn])


def make_ident(m, dtype, name="ident"):
    idf = m.sb(name + "f", [128, 128], F32)
    m.op("pool", lambda h: h.memset(idf[:], 1.0), writes=[idf])
    m.op("pool", lambda h: h.affine_select(out=idf[:], in_=idf[:], pattern=[[-1, 128]],
                                           compare_op=ALU.is_equal, fill=0.0, base=0,
                                           channel_multiplier=1), reads=[idf], writes=[idf])
    if dtype == F32:
        return idf
    idb = m.sb(name + "b", [128, 128], dtype)
    m.op("dve", lambda h: h.tensor_copy(out=idb[:], in_=idf[:]), reads=[idf], writes=[idb])
    return idb


def modulate(m, xT, modsb, l, hT, Tn):
    sh = m.sb("sh", [128, 16], F32)
    sc1 = m.sb("sc1", [128, 16], F32)
    m.op("dve", lambda h: h.tensor_copy(out=sh[:], in_=modsb[:, l, 0:16]), reads=[modsb], writes=[sh])
    m.op("dve", lambda h: h.tensor_scalar(out=sc1[:], in0=modsb[:, l, 16:32], scalar1=1.0, scalar2=None, op0=ALU.add),
         reads=[modsb], writes=[sc1])
    xs = [m.sb("xs%d" % i, [128, 16, 256], F32) for i in range(2)]
    for ci, t0 in enumerate(range(0, Tn, 256)):
        xb = xs[ci % 2]
        m.dma("sp", xb[:, 0:8, :], xT[:, 0:8, t0:t0 + 256], reads=[xT], writes=[xb])
        m.dma("sp", xb[:, 8:16, :], xT[:, 8:16, t0:t0 + 256], reads=[xT], writes=[xb])
        for kc in range(16):
            m.op("dve", lambda h, kc=kc, xb=xb, t0=t0: h.tensor_scalar(
                out=hT[:, kc, t0:t0 + 256], in0=xb[:, kc, :], scalar1=sc1[:, kc:kc + 1],
                scalar2=sh[:, kc:kc + 1], op0=ALU.mult, op1=ALU.add),
                reads=[xb, sc1, sh], writes=[hT])


def rmsnorm_T(m, srcT, KC, nfeat, gT, dstT, Tn, onesf, tagname):
    ps = m.ps(tagname + "ps", [128, 512])
    sq = [m.sb(tagname + "sq%d" % i, [128, 512], F32) for i in range(2)]
    rs = m.sb(tagname + "rs", [128, 512], F32)
    epsb = m.sb(tagname + "eps", [128, 1], F32)
    m.op("dve", lambda h: h.memset(epsb[:], LN_EPS), writes=[epsb])
    for t0 in range(0, Tn, 512):
        for kc in range(KC):
            s = sq[kc % 2]
            m.op("act", lambda h, s=s, kc=kc, t0=t0: h.activation(out=s[:], in_=srcT[:, kc, t0:t0 + 512], func=AF.Square),
                 reads=[srcT], writes=[s])
            mm(m, ps, ps[:, :], onesf, onesf[:, :], s, s[:, :], kc == 0, kc == KC - 1)
        m.op("act", lambda h: h.activation(out=rs[:], in_=ps[:, :], func=AF.Sqrt, bias=epsb[:], scale=1.0 / nfeat),
             reads=[ps, epsb], writes=[rs])
        m.op("dve", lambda h: h.reciprocal(out=rs[:], in_=rs[:]), reads=[rs], writes=[rs])
        for kc in range(KC):
            m.op("dve", lambda h, kc=kc, t0=t0: h.scalar_tensor_tensor(
                out=dstT[:, kc, t0:t0 + 512], in0=srcT[:, kc, t0:t0 + 512], scalar=gT[:, kc:kc + 1],
                in1=rs[:], op0=ALU.mult, op1=ALU.mult), reads=[srcT, gT, rs], writes=[dstT])


def evac_copy(m, eng, dst_b, dst_ap, ps_b, ps_ap, scale=None, func=None):
    if eng == "act":
        f = func if func is not None else AF.Copy
        if scale is None:
            m.op("act", lambda h: h.activation(out=dst_ap, in_=ps_ap, func=f), reads=[ps_b], writes=[dst_b])
        else:
            m.op("act", lambda h: h.activation(out=dst_ap, in_=ps_ap, func=f, scale=scale), reads=[ps_b], writes=[dst_b])
    else:
        if scale is None:
            m.op("dve", lambda h: h.tensor_copy(out=dst_ap, in_=ps_ap), reads=[ps_b], writes=[dst_b])
        else:
            m.op("dve", lambda h: h.tensor_scalar(out=dst_ap, in0=ps_ap, scalar1=scale, scalar2=None, op0=ALU.mult),
                 reads=[ps_b], writes=[dst_b])


class Stager:
    def __init__(self, m, n=4):
        self.m = m
        self.stg = [m.sb("stg%d" % i, [128, 512], BF16) for i in range(n)]
        self.k = 0

    def put(self, dst, dst_ap, ps, ps_ap, mr, eng, func=None, scale=None):
        m = self.m
        s = self.stg[self.k % len(self.stg)]
        self.k += 1
        evac_copy(m, eng, s, s[0:mr, :], ps, ps_ap, scale=scale, func=func)
        m.dma("sp", dst_ap, s[0:mr, :], reads=[s], writes=[dst])


def emit_M(m, E, modsb, SC):
    m.phase_begin()
    cT, aw, ab = E["cT"], E["aw"], E["ab"]
    c_sb = m.sb("c_sb", [128, 16], F32)
    sg = m.sb("sg", [128, 16], F32)
    cact = m.sb("cact", [128, 16, 2], F32)
    ab_sb = m.sb("ab_sb", [128, 24], F32)
    res = m.sb("res", [128, 24], F32)
    ws = [m.sb("w%d" % i, [128, 16, 128], F32) for i in range(2)]
    ps = m.ps("ps", [128, 512])
    m.dma("sp", c_sb[:], cT[:, :], reads=[cT], writes=[c_sb])
    m.dma("sp", ab_sb[:], ab[:, :], reads=[ab], writes=[ab_sb])
    m.op("act", lambda h: h.activation(out=sg[:], in_=c_sb[:], func=AF.Sigmoid), reads=[c_sb], writes=[sg])
    for j in range(2):
        m.op("dve", lambda h, j=j: h.tensor_tensor(out=cact[:, :, j], in0=sg[:], in1=c_sb[:], op=ALU.mult),
             reads=[sg, c_sb], writes=[cact])
    for g in range(24):
        w = ws[g % 2]
        m.dma("sp", w[:, 0:8, :], aw[g, :, 0:8, :], reads=[aw], writes=[w])
        m.dma("sp", w[:, 8:16, :], aw[g, :, 8:16, :], reads=[aw], writes=[w])
        for kc in range(16):
            mm(m, ps, ps[:, 2 * g:2 * g + 2], w, w[:, kc, :], cact, cact[:, kc, :], kc == 0, kc == 15)
    psv = ps[:, 0:48].rearrange("p (g two) -> p g two", two=2)
    m.op("dve", lambda h: h.tensor_tensor(out=res[:], in0=psv[:, :, 0], in1=ab_sb[:], op=ALU.add),
         reads=[ps, ab_sb], writes=[res])
    m.dma("sp", SC["mod_s"][:, :], res[:], reads=[res], writes=[SC["mod_s"]])
    m.allgather(SC["mod_s"], SC["mod_g"])
    gsrc = SC["mod_g"].t.rearrange("(r p) (l q) -> r p l q", p=128, q=6)
    for r in range(NCORES):
        m.dma("sp", modsb[:, :, r * 6:(r + 1) * 6], gsrc[r], reads=[SC["mod_g"]], writes=[modsb])
    m.phase_end()


def emit_P_dsa(m, E, j, l, xT, modsb, SC):
    m.phase_begin()
    w_in, w_uq, w_qi, w_uk = (E[k % j] for k in ("dsa_w_in%d", "dsa_w_uq%d", "dsa_w_qi%d", "dsa_w_uk%d"))
    gqT, gkvT = E["dsa_gqT%d" % j], E["dsa_gkvT%d" % j]
    o_qlat, o_qidx, o_widx, o_sg = SC["qlatT"], SC["qidxT"], SC["widx"], SC["sgT"]
    s_ckv, s_kidx, s_v = SC["s_ckv"], SC["s_kidx"], SC["s_v"]
    hT = m.sb("hT", [128, 16, T], BF16)
    modulate(m, xT, modsb, l, hT, T)
    lin = LinT(m, 16)
    st = Stager(m)
    onesf = m.sb("onesf", [128, 128], F32)
    m.op("pool", lambda h: h.memset(onesf[:], 1.0), writes=[onesf])
    cqT = m.sb("cqT", [128, 4, T], F32)
    ckvT = m.sb("ckvT_s", [128, 2, T], F32)

    def ev_cq(fa, mr, t0, tn, ps, ps_ap):
        evac_copy(m, "act", cqT, cqT[:, fa // 128, t0:t0 + tn], ps, ps_ap)

    def ev_ckv(fa, mr, t0, tn, ps, ps_ap):
        evac_copy(m, "dve", ckvT, ckvT[:, (fa - 512) // 128, t0:t0 + tn], ps, ps_ap)

    def ev_kidx(fa, mr, t0, tn, ps, ps_ap):
        st.put(s_kidx, s_kidx[:, t0:t0 + tn], ps, ps_ap, mr, "dve")

    def ev_gate(fa, mr, t0, tn, ps, ps_ap):
        st.put(o_sg, o_sg[:, (fa - 912) // 128, t0:t0 + tn], ps, ps_ap, mr, "act", func=AF.Silu)

    lin.run(w_in, w_in, 0, 512, 16, hT, T, ev_cq)
    lin.run(w_in, w_in, 512, 256, 16, hT, T, ev_ckv)
    lin.run(w_in, w_in, 768, 128, 16, hT, T, ev_kidx)
    gq = m.sb("gq", [128, 4], F32)
    gkv = m.sb("gkv", [128, 2], F32)
    m.dma("sp", gq[:], gqT[:, :], reads=[gqT], writes=[gq])
    m.dma("sp", gkv[:], gkvT[:, :], reads=[gkvT], writes=[gkv])
    cqn = m.sb("cqn", [128, 4, T], BF16)
    ckvn = m.sb("ckvn", [128, 2, T], BF16)
    rmsnorm_T(m, ckvT, 2, 256.0, gkv, ckvn, T, onesf, "rk")
    for cc in range(2):
        m.dma("sp", s_ckv[cc * 128:(cc + 1) * 128, :], ckvn[:, cc, :], reads=[ckvn], writes=[s_ckv])
    identb = make_ident(m, BF16)
    vt = m.sb("vt", [128, 8, 256], BF16)
    ptp = [m.ps("ptp%d" % i, [128, 512], BF16) for i in range(2)]
    for jj in range(8):
        pp = ptp[jj % 2]
        for cc in range(2):
            m.op("pe", lambda h, jj=jj, cc=cc, pp=pp: h.transpose(pp[:, cc * 128:(cc + 1) * 128], ckvn[:, cc, jj * 128:(jj + 1) * 128], identb[:]),
                 reads=[ckvn, identb], writes=[pp])
        evac_copy(m, "act" if jj % 2 else "dve", vt, vt[:, jj, :], pp, pp[:, 0:256])
    m.dma("sp", s_v.t.rearrange("(j i) c -> i j c", i=128), vt[:], reads=[vt], writes=[s_v])
    m.allgather(s_ckv, SC["g_ckv"])
    m.allgather(s_kidx, SC["g_kidx"])
    m.allgather(s_v, SC["g_v"])
    rmsnorm_T(m, cqT, 4, 512.0, gq, cqn, T, onesf, "rq")
    lin.run(w_in, w_in, 912, 2048, 16, hT, T, ev_gate)
    slot = lin.load(w_in, w_in, 896, 16, 16)
    wst = m.sb("wst", [128, 8, 16], F32)
    for jj in range(8):
        ps = lin.banks[jj % 2]
        for kc in range(16):
            mm(m, ps, ps[:, 0:16], hT, hT[:, kc, jj * 128:(jj + 1) * 128], slot, slot[:, kc, 0:16], kc == 0, kc == 15)
        evac_copy(m, "dve", wst, wst[:, jj, :], ps, ps[:, 0:16])
    m.dma("sp", o_widx[:, :, :], wst[:], reads=[wst], writes=[o_widx])
    qT = m.sb("qT", [128, 16, T], BF16)

    def ev_q(fa, mr, t0, tn, ps, ps_ap):
        evac_copy(m, "act", qT, qT[:, fa // 128, t0:t0 + tn], ps, ps_ap)

    lin.run(w_uq, w_uq, 0, 2048, 4, cqn, T, ev_q)
    wuk = m.sb("wuk", [128, 16, 256], BF16)
    m.dma("pool", wuk[:, 0:8, :], w_uk[0:8].rearrange("h d c -> d h c"), reads=[w_uk], writes=[wuk])
    m.dma("pool", wuk[:, 8:16, :], w_uk[8:16].rearrange("h d c -> d h c"), reads=[w_uk], writes=[wuk])
    nb = 0
    for hh in range(16):
        for cc in range(2):
            for t0 in range(0, T, 512):
                ps = lin.banks[nb % 2]
                nb += 1
                mm(m, ps, ps[:, :], wuk, wuk[:, hh, cc * 128:(cc + 1) * 128], qT, qT[:, hh, t0:t0 + 512], True, True)
                st.put(o_qlat, o_qlat[:, cc, hh, t0:t0 + 512], ps, ps[:, :], 128, "act" if nb % 2 else "dve", scale=SCALE)

    def ev_qi(fa, mr, t0, tn, ps, ps_ap):
        st.put(o_qidx, o_qidx[:, fa // 128, t0:t0 + tn], ps, ps_ap, mr, "dve")

    lin.run(w_qi, w_qi, 0, 2048, 4, cqn, T, ev_qi)
    m.phase_end()


class AttnBufs:
    def __init__(self, m, vw):
        self.m = m
        self.vw = vw
        self.S = [m.ps("S%d" % i, [128, 512]) for i in range(2)]
        self.PT = [m.ps("PT%d" % i, [128, 1024], BF16) for i in range(2)]
        self.acc = [m.ps("acc%d" % i, [128, 512]) for i in range(2)]
        self.PTf = m.ps("PTf", [128, 1024], BF16)
        self.X = m.ps("X", [128, 512])
        self.sc = [m.sb("sc%d" % i, [128, 512], F32) for i in range(3)]
        self.p = [m.sb("p%d" % i, [128, 512], BF16) for i in range(3)]
        self.pt = [m.sb("pt%d" % i, [128, 512], BF16) for i in range(3)]
        self.ost = [m.sb("ost%d" % i, [128, vw], BF16) for i in range(2)]
        self.otr = [m.sb("otr%d" % i, [128, vw], BF16) for i in range(2)]
        self.rden = [m.sb("rden%d" % i, [128, 1], F32) for i in range(2)]
        self.ident = make_ident(m, BF16)
        self.n = 0
        self.na = 0


class Tile:
    def __init__(self, m, A, acc, score_fn, dm_b, dm_ap, slope, exp_fn, v_b, v_aps, first, last, pre=None, fin=None):
        self.m, self.Ab, self.acc = m, A, acc
        self.score_fn, self.dm_b, self.dm_ap, self.slope, self.exp_fn = score_fn, dm_b, dm_ap, slope, exp_fn
        self.v_b, self.v_aps, self.first, self.last = v_b, v_aps, first, last
        self.pre, self.fin = pre, fin
        n = A.n
        A.n += 1
        self.n = n
        self.Sb, self.PTb = A.S[n % 2], A.PT[n % 2]
        self.sc, self.p, self.pt = A.sc[n % 3], A.p[n % 3], A.pt[n % 3]

    def stage_a(self):
        m = self.m
        if self.pre is not None:
            self.pre()
        Sb, sc, p = self.Sb, self.sc, self.p
        self.score_fn(Sb)
        dm_b, dm_ap, slope = self.dm_b, self.dm_ap, self.slope
        m.op("dve", lambda h: h.scalar_tensor_tensor(out=sc[:], in0=dm_ap, scalar=slope, in1=Sb[:, :],
                                                     op0=ALU.mult, op1=ALU.add), reads=[dm_b, Sb], writes=[sc])
        self.exp_fn(sc, p)

    def stage_b(self):
        m, A = self.m, self.Ab
        p, PT, pt = self.p, self.PTb, self.pt
        for q in range(4):
            m.op("pe", lambda h, q=q: h.transpose(PT[:, q * 128:(q + 1) * 128], p[:, q * 128:(q + 1) * 128], A.ident[:]),
                 reads=[p, A.ident], writes=[PT])
        if self.n % 2 == 0:
            m.op("act", lambda h: h.activation(out=pt[:], in_=PT[:, 0:512], func=AF.Copy), reads=[PT], writes=[pt])
        else:
            m.op("dve", lambda h: h.tensor_copy(out=pt[:], in_=PT[:, 0:512]), reads=[PT], writes=[pt])

    def stage_c(self):
        m, A, acc, pt = self.m, self.Ab, self.acc, self.pt
        vw = A.vw
        for q in range(4):
            mm(m, acc, acc[:, 0:vw + 1], pt, pt[:, q * 128:(q + 1) * 128], self.v_b, self.v_aps[q],
               self.first and q == 0, self.last and q == 3)


def run_pipeline(tiles):
    n = len(tiles)
    deferred = []
    for k in range(n + 3):
        if k < n:
            tiles[k].stage_a()
        if 0 <= k - 1 < n:
            tiles[k - 1].stage_b()
        todo, deferred = deferred, []
        for f in todo:
            f()
        if 0 <= k - 2 < n:
            t = tiles[k - 2]
            t.stage_c()
            if t.fin is not None:
                deferred.append(t.fin())


def attn_finish(m, A, acc, oT, hh, j):
    k = A.na
    A.na += 1
    vw = A.vw
    nvc = vw // 128
    rd, ost, otr = A.rden[k % 2], A.ost[k % 2], A.otr[k % 2]
    PT = A.PTf
    m.op("dve", lambda h: h.reciprocal(out=rd[:], in_=acc[:, vw:vw + 1]), reads=[acc], writes=[rd])
    m.op("act", lambda h: h.activation(out=ost[:], in_=acc[:, 0:vw], func=AF.Copy, scale=rd[:]),
         reads=[acc, rd], writes=[ost])

    def stage2():
        for vc in range(nvc):
            m.op("pe", lambda h, vc=vc: h.transpose(PT[:, vc * 128:(vc + 1) * 128], ost[:, vc * 128:(vc + 1) * 128], A.ident[:]),
                 reads=[ost, A.ident], writes=[PT])
        m.op("dve", lambda h: h.tensor_copy(out=otr[:], in_=PT[:, 0:vw]), reads=[PT], writes=[otr])
        m.dma("sp", oT[:, hh * nvc:(hh + 1) * nvc, j * 128:(j + 1) * 128], otr[:, :].rearrange("p (v t) -> p v t", t=128),
              reads=[otr], writes=[oT])
    return stage2


def emit_B_dsa(m, E, SC):
    m.phase_begin()
    i_qlat, i_qidx, i_widx = SC["qlatT"], SC["qidxT"], SC["widx"]
    g_ckv, g_kidx, g_v = SC["g_ckv"], SC["g_kidx"], SC["g_v"]
    oT = SC["oT_dsa"]
    ckv = m.sb("ckv", [128, 2, S], BF16)
    vaug = m.sb("vaug", [128, 64, 257], BF16)
    kidx = m.sb("kidx", [128, S], BF16)
    score = m.sb("score", [128, S], F32)
    sv = [m.view(score, "score%d" % k) for k in range(16)]
    junk = m.sb("junk", [128, S], U8)
    widx = m.sb("widx_s", [128, 8, 16], F32)
    tpos = m.sb("tpos_s", [128, 8], F32)
    slopes = m.sb("slopes_s", [128, 16], F32)
    pow2 = m.sb("pow2_s", [128, NBIS], F32)
    iota = m.sb("iota_s", [128, 512], F32)
    for dst, srcb in ((widx, i_widx), (tpos, E["tpos"]), (slopes, E["slopes"]), (pow2, E["pow2"]), (iota, E["iota"])):
        m.dma("sp", dst[:], srcb[:], reads=[srcb], writes=[dst])
    for r in range(NCORES):
        for cc in range(2):
            m.dma("sp", ckv[:, cc, :].rearrange("p (j r i) -> p j r i", r=8, i=128)[:, :, r, :],
                  g_ckv[r * 256 + cc * 128:r * 256 + (cc + 1) * 128, :].rearrange("p (j i) -> p j i", i=128),
                  reads=[g_ckv], writes=[ckv])
        m.dma("sp", kidx[:, :].rearrange("p (j r i) -> p j r i", r=8, i=128)[:, :, r, :],
              g_kidx[r * 128:(r + 1) * 128, :].rearrange("p (j i) -> p j i", i=128), reads=[g_kidx], writes=[kidx])
    m.op("pool", lambda h: h.memset(vaug[:], 1.0), writes=[vaug])
    for r in range(NCORES):
        m.dma("sp", vaug[:, :, :].rearrange("p (j r) c -> p j r c", r=8)[:, :, r, 0:256],
              g_v[r * T:(r + 1) * T, :].rearrange("(j i) c -> i j c", i=128), reads=[g_v], writes=[vaug])

    A = AttnBufs(m, 256)
    Lb = A.S
    tmp = [m.sb("tmp%d" % i, [128, 512], F32) for i in range(3)]
    qlat = [m.sb("qlat%d" % i, [128, 2, 16, 128], BF16) for i in range(2)]
    qidx = [m.sb("qidx%d" % i, [128, 16, 128], BF16) for i in range(2)]
    small = {k: m.sb("sm_" + k, [128, 1], F32) for k in ("habs", "lo", "mid", "cnt", "g", "gneg")}
    steps = m.sb("steps", [128, NBIS], F32)
    gm = m.sb("gm", [128, 16], F32)
    G = m.sb("G", [128, 16], F32)
    nL = 0
    nT = 0
    for j in range(NSLOT):
        NK = 2 * j + 2
        N = NK * 512
        ql, qi = qlat[j % 2], qidx[j % 2]
        for cc in range(2):
            m.dma("sp", ql[:, cc, :, :], i_qlat[:, cc, :, j * 128:(j + 1) * 128], reads=[i_qlat], writes=[ql])
        m.dma("sp", qi[:, :, :], i_qidx[:, :, j * 128:(j + 1) * 128], reads=[i_qidx], writes=[qi])
        for kt in range(NK):
            ks = slice(kt * 512, (kt + 1) * 512)
            for hh in range(16):
                L = Lb[nL % 2]
                nL += 1
                mm(m, L, L[:, :], qi, qi[:, hh, :], kidx, kidx[:, ks], True, True)
                if hh == 0:
                    m.op("dve", lambda h, L=L, ks=ks, j=j: h.tensor_scalar(
                        out=score[:, ks], in0=L[:, :], scalar1=0.0, scalar2=widx[:, j, 0:1], op0=ALU.max, op1=ALU.mult),
                        reads=[L, widx], writes=[sv[kt]])
                else:
                    tb = tmp[nT % 3]
                    nT += 1
                    m.op("dve", lambda h, L=L, tb=tb, j=j, hh=hh: h.tensor_scalar(
                        out=tb[:], in0=L[:, :], scalar1=0.0, scalar2=widx[:, j, hh:hh + 1], op0=ALU.max, op1=ALU.mult),
                        reads=[L, widx], writes=[tb])
                    m.op("pool", lambda h, tb=tb, ks=ks: h.tensor_tensor(out=score[:, ks], in0=score[:, ks], in1=tb[:], op=ALU.add),
                         reads=[tb, sv[kt]], writes=[sv[kt]])
        habs, lo, mid, cnt, g, gneg = (small[k] for k in ("habs", "lo", "mid", "cnt", "g", "gneg"))
        m.op("dve", lambda h, N=N: h.tensor_reduce(out=habs[:], in_=score[:, 0:N], axis=AX.X, op=ALU.max, apply_absolute_value=True),
             reads=sv[0:NK], writes=[habs])
        for kt in (2 * j, 2 * j + 1):
            ks = slice(kt * 512, (kt + 1) * 512)
            tb = tmp[nT % 3]
            nT += 1
            m.op("dve", lambda h, tb=tb, j=j, kt=kt: h.tensor_scalar(
                out=tb[:], in0=iota[:], scalar1=tpos[:, j:j + 1], scalar2=float(kt * 512), op0=ALU.subtract, op1=ALU.add),
                reads=[iota, tpos], writes=[tb])
            m.op("dve", lambda h, tb=tb: h.tensor_scalar(out=tb[:], in0=tb[:], scalar1=0.0, scalar2=-1.0e30, op0=ALU.is_gt, op1=ALU.mult),
                 reads=[tb], writes=[tb])
            m.op("dve", lambda h, tb=tb, ks=ks: h.tensor_tensor(out=score[:, ks], in0=score[:, ks], in1=tb[:], op=ALU.add),
                 reads=[tb, sv[kt]], writes=[sv[kt]])
        m.op("dve", lambda h: h.tensor_scalar(out=steps[:], in0=pow2[:], scalar1=habs[:], scalar2=None, op0=ALU.mult),
             reads=[pow2, habs], writes=[steps])
        m.op("dve", lambda h: h.tensor_scalar(out=lo[:], in0=habs[:], scalar1=-1.0, scalar2=None, op0=ALU.mult),
             reads=[habs], writes=[lo])
        for k in range(NBIS):
            m.op("dve", lambda h, k=k: h.tensor_tensor(out=mid[:], in0=lo[:], in1=steps[:, k:k + 1], op=ALU.add),
                 reads=[lo, steps], writes=[mid])
            m.op("dve", lambda h, N=N: h.tensor_scalar(out=junk[:, 0:N], in0=score[:, 0:N], scalar1=mid[:], scalar2=None,
                                                       op0=ALU.is_ge, op1=ALU.add, accum_out=cnt[:]),
                 reads=sv[0:NK] + [mid], writes=[junk, cnt])
            m.op("dve", lambda h, k=k: h.tensor_scalar(out=g[:], in0=cnt[:], scalar1=255.5, scalar2=steps[:, k:k + 1],
                                                       op0=ALU.is_gt, op1=ALU.mult), reads=[cnt, steps], writes=[g])
            m.op("dve", lambda h: h.tensor_tensor(out=lo[:], in0=lo[:], in1=g[:], op=ALU.add), reads=[lo, g], writes=[lo])
        for kt in range(NK):
            ks = slice(kt * 512, (kt + 1) * 512)
            tb = tmp[nT % 3]
            nT += 1
            m.op("dve", lambda h, ks=ks: h.tensor_scalar(out=score[:, ks], in0=score[:, ks], scalar1=lo[:], scalar2=BIGD,
                                                         op0=ALU.is_ge, op1=ALU.mult), reads=[sv[kt], lo], writes=[sv[kt]])
            m.op("dve", lambda h, tb=tb, j=j, kt=kt: h.tensor_scalar(
                out=tb[:], in0=iota[:], scalar1=tpos[:, j:j + 1], scalar2=float(kt * 512) - BIGD, op0=ALU.subtract, op1=ALU.add),
                reads=[iota, tpos], writes=[tb])
            m.op("pool", lambda h, tb=tb, ks=ks: h.tensor_tensor(out=score[:, ks], in0=score[:, ks], in1=tb[:], op=ALU.add),
                 reads=[tb, sv[kt]], writes=[sv[kt]])
            m.op("dve", lambda h, ks=ks, kt=kt: h.tensor_reduce(out=gm[:, kt:kt + 1], in_=score[:, ks], axis=AX.X, op=ALU.max),
                 reads=[sv[kt]], writes=[gm])
        m.op("dve", lambda h, NK=NK: h.tensor_reduce(out=gneg[:], in_=gm[:, 0:NK], axis=AX.X, op=ALU.max), reads=[gm], writes=[gneg])
        m.op("dve", lambda h: h.tensor_scalar(out=G[:], in0=slopes[:], scalar1=gneg[:], scalar2=-1.0, op0=ALU.mult, op1=ALU.mult),
             reads=[slopes, gneg], writes=[G])
        tiles = []
        for hh in range(16):
            acc = A.acc[hh % 2]
            for kt in range(NK):
                ks = slice(kt * 512, (kt + 1) * 512)

                def score_fn(Sb, hh=hh, ks=ks, ql=ql):
                    mm(m, Sb, Sb[:, :], ql, ql[:, 0, hh, :], ckv, ckv[:, 0, ks], True, False)
                    mm(m, Sb, Sb[:, :], ql, ql[:, 1, hh, :], ckv, ckv[:, 1, ks], False, True)

                def exp_fn(sc, p, hh=hh):
                    m.op("act", lambda h: h.activation(out=p[:], in_=sc[:], func=AF.Exp, bias=G[:, hh:hh + 1], scale=1.0),
                         reads=[sc, G], writes=[p])

                fin = None
                if kt == NK - 1:
                    fin = (lambda acc=acc, hh=hh, j=j: attn_finish(m, A, acc, oT, hh, j))
                tiles.append(Tile(m, A, acc, score_fn, sv[kt], score[:, ks], SLOPES[hh], exp_fn, vaug,
                                  [vaug[:, kt * 4 + q, :] for q in range(4)], kt == 0, kt == NK - 1, fin=fin))
        run_pipeline(tiles)
    m.phase_end()


def emit_Q(m, E, dsa, j, l, xT, xoT, modsb, SC):
    m.phase_begin()
    NO = 32 if dsa else 16
    i_o = SC["oT_dsa"] if dsa else SC["oT_moba"]
    i_sg = SC["sgT"]
    w_o = E[("dsa_w_o%d" if dsa else "moba_w_o%d") % j]
    lin = LinT(m, 16)
    onesf = m.sb("onesf", [128, 128], F32)
    m.op("pool", lambda h: h.memset(onesf[:], 1.0), writes=[onesf])
    gp = m.sb("gp", [128, 16], F32)
    lng = m.sb("lng", [128, 16], F32)
    lnb = m.sb("lnb", [128, 16], F32)
    epsb = m.sb("epsb", [128, 1], F32)
    m.op("dve", lambda h: h.memset(epsb[:], LN_EPS / (ALPHA * ALPHA)), writes=[epsb])
    m.dma("sp", lng[:], E["lngT"][:, l, :], reads=[E["lngT"]], writes=[lng])
    m.dma("sp", lnb[:], E["lnbT"][:, l, :], reads=[E["lnbT"]], writes=[lnb])
    m.op("dve", lambda h: h.tensor_scalar(out=gp[:], in0=modsb[:, l, 32:48], scalar1=1.0 / ALPHA, scalar2=None, op0=ALU.mult),
         reads=[modsb], writes=[gp])
    if dsa:
        i_wuv = E["dsa_w_uv%d" % j]
        wuv = m.sb("wuv", [128, 16, 2, 128], BF16)
        for cc in range(2):
            m.dma("pool", wuv[:, :, cc, :], i_wuv[:, cc * 128:(cc + 1) * 128, :].rearrange("h p d -> p h d"),
                  reads=[i_wuv], writes=[wuv])
    obf = m.sb("obf", [128, NO, 512], BF16)
    sg = m.sb("sg", [128, 16, 512], BF16)
    og = m.sb("og", [128, 16, 512], BF16)
    z = m.sb("z", [128, 16, 512], F32)
    pso = [m.ps("pso%d" % i, [128, 512]) for i in range(2)]
    pstat = m.ps("pstat", [128, 512])
    mean = m.sb("mean", [128, 512], F32)
    rstd = m.sb("rstd", [128, 512], F32)
    sq = [m.sb("sq%d" % i, [128, 512], F32) for i in range(2)]
    for t0 in range(0, T, 512):
        ts = slice(t0, t0 + 512)
        step = NO // 2
        for a in range(0, NO, step):
            m.dma("sp", obf[:, a:a + step, :], i_o[:, a:a + step, ts], reads=[i_o], writes=[obf])
        m.dma("sp", sg[:, :, :], i_sg[:, :, ts], reads=[i_sg], writes=[sg])
        for a in range(0, 16, 8):
            m.dma("sp", z[:, a:a + 8, :], xT[:, a:a + 8, ts], reads=[xT], writes=[z])
        for hh in range(16):
            if dsa:
                ps = pso[hh % 2]
                for cc in range(2):
                    mm(m, ps, ps[:, :], wuv, wuv[:, hh, cc, :], obf, obf[:, hh * 2 + cc, :], cc == 0, cc == 1)
                m.op("dve", lambda h, ps=ps, hh=hh: h.tensor_tensor(out=og[:, hh, :], in0=ps[:, :], in1=sg[:, hh, :], op=ALU.mult),
                     reads=[ps, sg], writes=[og])
            else:
                m.op("dve", lambda h, hh=hh: h.tensor_tensor(out=og[:, hh, :], in0=obf[:, hh, :], in1=sg[:, hh, :], op=ALU.mult),
                     reads=[obf, sg], writes=[og])

        def ev_y(fa, mr, tt0, tn, ps, ps_ap):
            kc = fa // 128
            m.op("dve", lambda h: h.scalar_tensor_tensor(out=z[:, kc, :], in0=ps_ap, scalar=gp[:, kc:kc + 1], in1=z[:, kc, :],
                                                         op0=ALU.mult, op1=ALU.add), reads=[ps, gp, z], writes=[z])

        lin.run(w_o, w_o, 0, 2048, 16, og, 512, ev_y)
        for kc in range(16):
            mm(m, pstat, pstat[:, :], onesf, onesf[:, :], z, z[:, kc, :], kc == 0, kc == 15)
        m.op("act", lambda h: h.activation(out=mean[:], in_=pstat[:, :], func=AF.Copy, scale=1.0 / D), reads=[pstat], writes=[mean])
        for kc in range(16):
            m.op("dve", lambda h, kc=kc: h.tensor_tensor(out=z[:, kc, :], in0=z[:, kc, :], in1=mean[:], op=ALU.subtract),
                 reads=[z, mean], writes=[z])
        for kc in range(16):
            s = sq[kc % 2]
            m.op("act", lambda h, s=s, kc=kc: h.activation(out=s[:], in_=z[:, kc, :], func=AF.Square), reads=[z], writes=[s])
            mm(m, pstat, pstat[:, :], onesf, onesf[:, :], s, s[:, :], kc == 0, kc == 15)
        m.op("act", lambda h: h.activation(out=rstd[:], in_=pstat[:, :], func=AF.Sqrt, bias=epsb[:], scale=1.0 / D),
             reads=[pstat, epsb], writes=[rstd])
        m.op("dve", lambda h: h.reciprocal(out=rstd[:], in_=rstd[:]), reads=[rstd], writes=[rstd])
        for kc in range(16):
            s = sq[kc % 2]
            m.op("dve", lambda h, s=s, kc=kc: h.scalar_tensor_tensor(out=s[:], in0=z[:, kc, :], scalar=lng[:, kc:kc + 1], in1=rstd[:],
                                                                     op0=ALU.mult, op1=ALU.mult), reads=[z, lng, rstd], writes=[s])
            m.op("act", lambda h, s=s, kc=kc: h.activation(out=z[:, kc, :], in_=s[:], func=AF.Identity, bias=lnb[:, kc:kc + 1], scale=1.0),
                 reads=[s, lnb, z], writes=[z])
        for a in range(0, 16, 8):
            m.dma("sp", xoT[:, a:a + 8, ts], z[:, a:a + 8, :], reads=[z], writes=[xoT])
    m.phase_end()


def emit_P_moba(m, E, j, l, xT, modsb, SC):
    m.phase_begin()
    w_in = E["moba_w_in%d" % j]
    o_q, s_k, s_v, o_sg = SC["qT"], SC["s_k"], SC["s_vm"], SC["sgT"]
    hT = m.sb("hT", [128, 16, T], BF16)
    modulate(m, xT, modsb, l, hT, T)
    lin = LinT(m, 16)
    st = Stager(m)

    def ev_q(fa, mr, t0, tn, ps, ps_ap):
        st.put(o_q, o_q[:, fa // 128, t0:t0 + tn], ps, ps_ap, mr, "act" if (fa // 128) % 2 else "dve", scale=SCALE)

    def ev_k(fa, mr, t0, tn, ps, ps_ap):
        st.put(s_k, s_k[fa - 2048:fa - 2048 + mr, t0:t0 + tn], ps, ps_ap, mr, "act" if (fa // 128) % 2 else "dve")

    def ev_g(fa, mr, t0, tn, ps, ps_ap):
        st.put(o_sg, o_sg[:, (fa - 6144) // 128, t0:t0 + tn], ps, ps_ap, mr, "act", func=AF.Silu)

    lin.run(w_in, w_in, 2048, 2048, 16, hT, T, ev_k)
    m.allgather(s_k, SC["g_k"])
    nb = 0
    sv_view = s_v.t.rearrange("(j i) f -> i j f", i=128)
    for fb in range(4):
        slot = lin.load(w_in, w_in, 4096 + fb * 512, 512, 16)
        for jj in range(8):
            ps = lin.banks[nb % 2]
            nb += 1
            for kc in range(16):
                mm(m, ps, ps[:, :], hT, hT[:, kc, jj * 128:(jj + 1) * 128], slot, slot[:, kc, :], kc == 0, kc == 15)
            st.put(s_v, sv_view[:, jj, fb * 512:(fb + 1) * 512], ps, ps[:, :], 128, "act" if nb % 2 else "dve")
    m.allgather(s_v, SC["g_vm"])
    lin.run(w_in, w_in, 0, 2048, 16, hT, T, ev_q)
    lin.run(w_in, w_in, 6144, 2048, 16, hT, T, ev_g)
    m.phase_end()


def emit_B_moba(m, E, SC):
    m.phase_begin()
    i_q, g_k, g_v = SC["qT"], SC["g_k"], SC["g_vm"]
    oT = SC["oT_moba"]
    tpos = m.sb("tpos_s", [128, 8], F32)
    own = m.sb("own_s", [128, 8], F32)
    iota = m.sb("iota_s", [128, 512], F32)
    blkend = m.sb("blkend_s", [128, 32], F32)
    s0row = m.sb("s0row_s", [128, 8, 32], F32)
    for dst, srcb in ((tpos, E["tpos"]), (own, E["ownstart"]), (iota, E["iota"]), (blkend, E["blkend"]), (s0row, E["s0row"])):
        m.dma("sp", dst[:], srcb[:], reads=[srcb], writes=[dst])
    dpast = m.sb("dpast", [128, 8, 512], F32)
    ddiag = m.sb("ddiag", [128, 8, 2, 512], F32)
    pastpen = m.sb("pastpen", [128, 8, 32], F32)
    pastf = m.sb("pastf", [128, 8, 32], F32)
    tcz = m.sb("tcz", [128, 512], F32)
    for j in range(8):
        m.op("dve", lambda h, j=j: h.tensor_scalar(out=dpast[:, j, :], in0=iota[:], scalar1=tpos[:, j:j + 1], scalar2=None,
                                                   op0=ALU.subtract), reads=[iota, tpos], writes=[dpast])
        for u in range(2):
            kt = 2 * j + u
            m.op("dve", lambda h, j=j, u=u, kt=kt: h.tensor_scalar(out=ddiag[:, j, u, :], in0=iota[:], scalar1=tpos[:, j:j + 1],
                                                                  scalar2=float(kt * 512), op0=ALU.subtract, op1=ALU.add),
                 reads=[iota, tpos], writes=[ddiag])
            m.op("dve", lambda h, j=j, u=u: h.tensor_scalar(out=tcz[:], in0=ddiag[:, j, u, :], scalar1=0.0, scalar2=-BIGD,
                                                            op0=ALU.is_gt, op1=ALU.mult), reads=[ddiag], writes=[tcz])
            m.op("dve", lambda h, j=j, u=u: h.tensor_tensor(out=ddiag[:, j, u, :], in0=ddiag[:, j, u, :], in1=tcz[:], op=ALU.add),
                 reads=[ddiag, tcz], writes=[ddiag])
        m.op("dve", lambda h, j=j: h.tensor_scalar(out=pastpen[:, j, :], in0=blkend[:], scalar1=own[:, j:j + 1], scalar2=-1.0e30,
                                                   op0=ALU.is_ge, op1=ALU.mult), reads=[blkend, own], writes=[pastpen])
        m.op("dve", lambda h, j=j: h.tensor_scalar(out=pastf[:, j, :], in0=blkend[:], scalar1=own[:, j:j + 1], scalar2=30000.0,
                                                   op0=ALU.is_lt, op1=ALU.mult), reads=[blkend, own], writes=[pastf])
    A = AttnBufs(m, 128)
    kh = [m.sb("kh%d" % i, [128, S], BF16) for i in range(2)]
    vh = [m.sb("vh%d" % i, [128, 64, 129], BF16) for i in range(2)]
    qh = [m.sb("qh%d" % i, [128, T], BF16) for i in range(2)]
    for v in vh:
        m.op("pool", lambda h, v=v: h.memset(v[:], 1.0), writes=[v])
    km = m.sb("km", [128, 32], F32)
    kmh = m.sb("kmh", [128, 32], BF16)
    kml = m.sb("kml", [128, 32], BF16)
    gp = m.sb("gp", [128, 32], F32)
    ga = m.sb("ga", [128, 32], F32)
    t1 = m.sb("t1", [128, 32], F32)
    mx = m.sb("mx", [128, 1], F32)
    BT = [m.sb("BT%d" % i, [128, 32], F32) for i in range(2)]
    ng = 0
    for hh in range(16):
        K_h, V_h, Q_h = kh[hh % 2], vh[hh % 2], qh[hh % 2]
        for r in range(NCORES):
            m.dma("sp", K_h[:, :].rearrange("p (j r i) -> p j r i", r=8, i=128)[:, :, r, :],
                  g_k[r * 2048 + hh * 128:r * 2048 + (hh + 1) * 128, :].rearrange("p (j i) -> p j i", i=128),
                  reads=[g_k], writes=[K_h])
            m.dma("sp", V_h[:, :, :].rearrange("p (j r) c -> p j r c", r=8)[:, :, r, 0:128],
                  g_v[r * T:(r + 1) * T, hh * 128:(hh + 1) * 128].rearrange("(j i) c -> i j c", i=128),
                  reads=[g_v], writes=[V_h])
        m.dma("sp", Q_h[:, :], i_q[:, hh, :], reads=[i_q], writes=[Q_h])
        m.op("dve", lambda h, K_h=K_h: h.tensor_reduce(out=km[:], in_=K_h[:, :].rearrange("p (n s) -> p n s", s=256), axis=AX.X, op=ALU.add),
             reads=[K_h], writes=[km])
        m.op("dve", lambda h: h.tensor_scalar(out=km[:], in0=km[:], scalar1=1.0 / 256, scalar2=None, op0=ALU.mult), reads=[km], writes=[km])
        m.op("dve", lambda h: h.tensor_copy(out=kmh[:], in_=km[:]), reads=[km], writes=[kmh])
        m.op("dve", lambda h: h.tensor_tensor(out=kml[:], in0=km[:], in1=kmh[:], op=ALU.subtract), reads=[km, kmh], writes=[kml])
        tiles = []
        for j in range(NSLOT):
            NK = 2 * j + 2
            qs = slice(j * 128, (j + 1) * 128)
            g_ps = A.X
            bt = BT[ng % 2]
            ng += 1

            def gate(j=j, qs=qs, g_ps=g_ps, bt=bt, hh=hh, Q_h=Q_h):
                mm(m, g_ps, g_ps[:, 0:32], Q_h, Q_h[:, qs], kmh, kmh[:], True, False)
                mm(m, g_ps, g_ps[:, 0:32], Q_h, Q_h[:, qs], kml, kml[:], False, True)
                m.op("dve", lambda h: h.tensor_tensor(out=gp[:], in0=g_ps[:, 0:32], in1=pastpen[:, j, :], op=ALU.add),
                     reads=[g_ps, pastpen], writes=[gp])
                srcb = gp
                for r in range(2):
                    m.op("dve", lambda h, srcb=srcb: h.tensor_reduce(out=mx[:], in_=srcb[:], axis=AX.X, op=ALU.max), reads=[srcb], writes=[mx])
                    m.op("dve", lambda h, srcb=srcb: h.tensor_scalar(out=t1[:], in0=srcb[:], scalar1=mx[:], scalar2=-1.0e30, op0=ALU.is_ge, op1=ALU.mult),
                         reads=[srcb, mx], writes=[t1])
                    m.op("dve", lambda h, srcb=srcb: h.tensor_tensor(out=ga[:], in0=srcb[:], in1=t1[:], op=ALU.add), reads=[srcb, t1], writes=[ga])
                    srcb = ga
                m.op("dve", lambda h: h.tensor_reduce(out=mx[:], in_=ga[:], axis=AX.X, op=ALU.max), reads=[ga], writes=[mx])
                m.op("dve", lambda h: h.tensor_scalar(out=t1[:], in0=gp[:], scalar1=mx[:], scalar2=1.0, op0=ALU.is_ge, op1=ALU.subtract),
                     reads=[gp, mx], writes=[t1])
                m.op("dve", lambda h: h.tensor_tensor(out=t1[:], in0=t1[:], in1=pastf[:, j, :], op=ALU.mult), reads=[t1, pastf], writes=[t1])
                m.op("dve", lambda h: h.scalar_tensor_tensor(out=bt[:], in0=s0row[:, j, :], scalar=SLOPES[hh], in1=t1[:],
                                                             op0=ALU.mult, op1=ALU.add), reads=[s0row, t1], writes=[bt])

            acc = A.acc[ng % 2]
            for kt in range(NK):
                ks = slice(kt * 512, (kt + 1) * 512)
                if kt < 2 * j:
                    dm_b, dm_ap = dpast, dpast[:, j, :]
                else:
                    dm_b, dm_ap = ddiag, ddiag[:, j, kt - 2 * j, :]

                def score_fn(Sb, ks=ks, qs=qs, Q_h=Q_h, K_h=K_h):
                    mm(m, Sb, Sb[:, :], Q_h, Q_h[:, qs], K_h, K_h[:, ks], True, True)

                def exp_fn(sc, p, kt=kt, bt=bt):
                    for u in range(2):
                        m.op("act", lambda h, u=u: h.activation(out=p[:, u * 256:(u + 1) * 256], in_=sc[:, u * 256:(u + 1) * 256], func=AF.Exp,
                                                                bias=bt[:, 2 * kt + u:2 * kt + u + 1], scale=1.0),
                             reads=[sc, bt], writes=[p])

                fin = None
                if kt == NK - 1:
                    fin = (lambda acc=acc, hh=hh, j=j: attn_finish(m, A, acc, oT, hh, j))
                tiles.append(Tile(m, A, acc, score_fn, dm_b, dm_ap, SLOPES[hh], exp_fn, V_h,
                                  [V_h[:, kt * 4 + q, :] for q in range(4)], kt == 0, kt == NK - 1,
                                  pre=gate if kt == 0 else None, fin=fin))
        run_pipeline(tiles)
    m.phase_end()


def build_all(nlayers=DEPTH, layers=None):
    nc = bass.Bass("TRN2", target_bir_lowering=False)
    m = MK(nc)
    E = {}
    E["xT"] = m.din("xT", [128, 16, T], F32)
    E["cT"] = m.din("cT", [128, 16], F32)
    E["aw"] = m.din("aw", [24, 128, 16, 128], F32)
    E["ab"] = m.din("ab", [128, 24], F32)
    E["lngT"] = m.din("lngT", [128, DEPTH, 16], F32)
    E["lnbT"] = m.din("lnbT", [128, DEPTH, 16], F32)
    for j in range(2):
        E["dsa_w_in%d" % j] = m.din("dsa_w_in%d" % j, [2048, 2960], F32)
        E["dsa_gqT%d" % j] = m.din("dsa_gqT%d" % j, [128, 4], F32)
        E["dsa_gkvT%d" % j] = m.din("dsa_gkvT%d" % j, [128, 2], F32)
        E["dsa_w_uq%d" % j] = m.din("dsa_w_uq%d" % j, [512, 2048], F32)
        E["dsa_w_qi%d" % j] = m.din("dsa_w_qi%d" % j, [512, 2048], F32)
        E["dsa_w_uk%d" % j] = m.din("dsa_w_uk%d" % j, [16, 128, 256], F32)
        E["dsa_w_uv%d" % j] = m.din("dsa_w_uv%d" % j, [16, 256, 128], F32)
        E["dsa_w_o%d" % j] = m.din("dsa_w_o%d" % j, [2048, 2048], F32)
        E["moba_w_in%d" % j] = m.din("moba_w_in%d" % j, [2048, 8192], F32)
        E["moba_w_o%d" % j] = m.din("moba_w_o%d" % j, [2048, 2048], F32)
    E["tpos"] = m.din("tpos", [128, 8], F32)
    E["ownstart"] = m.din("ownstart", [128, 8], F32)
    E["slopes"] = m.din("slopes", [128, 16], F32)
    E["pow2"] = m.din("pow2", [128, NBIS], F32)
    E["iota"] = m.din("iota", [128, 512], F32)
    E["blkend"] = m.din("blkend", [128, 32], F32)
    E["s0row"] = m.din("s0row", [128, 8, 32], F32)
    out = m.dout("xoT", [128, 16, T], F32)
    SC = {
        "mod_s": m.dram("mod_s", [128, 24], F32), "mod_g": m.dram("mod_g", [1024, 24], F32),
        "xa": m.dram("xa", [128, 16, T], F32), "xb": m.dram("xb", [128, 16, T], F32),
        "qlatT": m.dram("qlatT", [128, 2, 16, T], BF16), "qidxT": m.dram("qidxT", [128, 16, T], BF16),
        "widx": m.dram("widx", [128, 8, 16], F32), "sgT": m.dram("sgT", [128, 16, T], BF16),
        "s_ckv": m.dram("s_ckv", [256, T], BF16), "g_ckv": m.dram("g_ckv", [2048, T], BF16),
        "s_kidx": m.dram("s_kidx", [128, T], BF16), "g_kidx": m.dram("g_kidx", [1024, T], BF16),
        "s_v": m.dram("s_v", [T, 256], BF16), "g_v": m.dram("g_v", [S, 256], BF16),
        "oT_dsa": m.dram("oT_dsa", [128, 32, T], BF16), "oT_moba": m.dram("oT_moba", [128, 16, T], BF16),
        "qT": m.dram("qT", [128, 16, T], BF16),
        "s_k": m.dram("s_k", [2048, T], BF16), "g_k": m.dram("g_k", [8 * 2048, T], BF16),
        "s_vm": m.dram("s_vm", [T, 2048], BF16), "g_vm": m.dram("g_vm", [S, 2048], BF16),
    }
    modsb = m.sb("modsb", [128, DEPTH, 48], F32)
    emit_M(m, E, modsb, SC)
    xin = E["xT"]
    layers = list(range(nlayers)) if layers is None else layers
    for l in layers:
        j = l // 2
        xout = out if l == layers[-1] else (SC["xa"] if l % 2 == 0 else SC["xb"])
        if l % 2 == 0:
            emit_P_dsa(m, E, j, l, xin, modsb, SC)
            emit_B_dsa(m, E, SC)
            emit_Q(m, E, True, j, l, xin, xout, modsb, SC)
        else:
            emit_P_moba(m, E, j, l, xin, modsb, SC)
            emit_B_moba(m, E, SC)
            emit_Q(m, E, False, j, l, xin, xout, modsb, SC)
        xin = xout
    m.finish()
    return nc


_PROGS = {}


def prog(name, builder):
    if name not in _PROGS:
        _PROGS[name] = builder()
    return _PROGS[name]


def fm(a):
    F, Tn = a.shape
    return np.ascontiguousarray(a.reshape(F // 128, 128, Tn).transpose(1, 0, 2))


def unfm(a):
    P, KC, Tn = a.shape
    return np.ascontiguousarray(a.transpose(1, 0, 2).reshape(KC * 128, Tn))


def vecT(v):
    return np.ascontiguousarray(v.reshape(-1, 128).T)


def core_positions(core):
    j = np.arange(8)[:, None]
    i = np.arange(128)[None, :]
    return ((8 * j + core) * 128 + i).reshape(-1)


def kernel(x, c, ada_w, ada_b, ln_g, ln_b, dsa_w_in, dsa_g_q, dsa_g_kv, dsa_w_uq, dsa_w_qi,
           dsa_w_uk, dsa_w_uv, dsa_w_o, moba_w_in, moba_w_o, _nlayers=DEPTH):
    f32 = lambda a: np.ascontiguousarray(np.asarray(a, dtype=np.float32))
    x, c, ada_w, ada_b, ln_g, ln_b = map(f32, (x, c, ada_w, ada_b, ln_g, ln_b))
    dsa_w_in, dsa_g_q, dsa_g_kv, dsa_w_uq, dsa_w_qi = map(f32, (dsa_w_in, dsa_g_q, dsa_g_kv, dsa_w_uq, dsa_w_qi))
    dsa_w_uk, dsa_w_uv, dsa_w_o, moba_w_in, moba_w_o = map(f32, (dsa_w_uk, dsa_w_uv, dsa_w_o, moba_w_in, moba_w_o))
    nc = prog("all%d" % _nlayers, lambda: build_all(_nlayers))
    x0 = x[0]
    common = {
        "cT": vecT(c[0]),
        "lngT": np.ascontiguousarray(ln_g.reshape(DEPTH, 16, 128).transpose(2, 0, 1)),
        "lnbT": np.ascontiguousarray(ln_b.reshape(DEPTH, 16, 128).transpose(2, 0, 1)),
        "slopes": np.tile(np.asarray(SLOPES, np.float32)[None, :], (128, 1)),
        "pow2": np.tile((2.0 ** -np.arange(NBIS, dtype=np.float64)).astype(np.float32)[None, :], (128, 1)),
        "iota": np.tile(np.arange(512, dtype=np.float32)[None, :], (128, 1)),
        "blkend": np.tile((np.arange(32, dtype=np.float32) * 256 + 255)[None, :], (128, 1)),
    }
    s0row = np.zeros((128, 8, 32), np.float32)
    for j in range(8):
        for n in range(4 * j):
            s0row[:, j, n] = 512.0 * (n // 2)
    common["s0row"] = s0row
    for j in range(2):
        common["dsa_w_in%d" % j] = dsa_w_in[j]
        common["dsa_gqT%d" % j] = vecT(dsa_g_q[j])
        common["dsa_gkvT%d" % j] = vecT(dsa_g_kv[j])
        common["dsa_w_uq%d" % j] = dsa_w_uq[j]
        common["dsa_w_qi%d" % j] = dsa_w_qi[j]
        common["dsa_w_uk%d" % j] = dsa_w_uk[j]
        common["dsa_w_uv%d" % j] = dsa_w_uv[j]
        common["dsa_w_o%d" % j] = dsa_w_o[j]
        common["moba_w_in%d" % j] = moba_w_in[j]
        common["moba_w_o%d" % j] = moba_w_o[j]
    in_maps = []
    for core in range(NCORES):
        d = dict(common)
        pos = core_positions(core)
        d["xT"] = fm(np.ascontiguousarray(x0[pos].T))
        aw = np.empty((24, 128, 16, 128), np.float32)
        ab = np.empty((128, 24), np.float32)
        for l in range(DEPTH):
            for q in range(6):
                f0 = (core * 6 + q) * 128
                aw[l * 6 + q] = ada_w[l][:, f0:f0 + 128].reshape(16, 128, 128).transpose(1, 0, 2)
                ab[:, l * 6 + q] = ada_b[l][f0:f0 + 128]
        d["aw"], d["ab"] = aw, ab
        tp = pos.reshape(8, 128).T.astype(np.float32)
        d["tpos"] = np.ascontiguousarray(tp)
        d["ownstart"] = np.ascontiguousarray(np.floor(tp / 256.0).astype(np.float32) * 256.0)
        in_maps.append(d)
    res = run_bass_kernel_spmd(nc, in_maps, core_ids=list(range(NCORES))).results
    out = np.empty((1, S, D), np.float32)
    for core in range(NCORES):
        out[0, core_positions(core)] = unfm(res[core]["xoT"]).T
    return out
```
